# Optimizing a Trainium2 kernel written in Bass

```python
import math
import jax
import jax.numpy as jnp
from jax import lax
import numpy as np

D_MODEL = 2048
BATCH = 8
SEQ = 2048
DEPTH = 4

ATT_HEADS = 8
KV_RANK = 256
IDX_HEADS = 8
IDX_DIM = 64
TOPK_MAX = 256
Q_BLOCK = 128
REL_BUCKETS = 32
REL_MAX_DIST = 128
M_HEADS = 4
M_QK_DIM = 128
M_V_DIM = 256
M_CHUNK = 64
CONV_WIDTH = 4
D_FF = 5504
EPS = 1e-6

COL_WIDTHS = (
    ATT_HEADS * KV_RANK,
    KV_RANK,
    IDX_HEADS * IDX_DIM,
    IDX_DIM,
    IDX_HEADS,
    2 * M_HEADS * M_QK_DIM,
    M_HEADS * M_V_DIM,
    M_HEADS * M_V_DIM,
    M_HEADS,
    M_HEADS,
    D_MODEL,
    D_MODEL,
)
D_IN = sum(COL_WIDTHS)
SPLIT_POINTS = tuple(int(v) for v in np.cumsum(COL_WIDTHS)[:-1])

kernel_name = 'hybrid_dsa_mlstm_macaron_trunk'


def rms_norm(x, g):
    xf = x.astype(jnp.float32)
    y = xf * lax.rsqrt(jnp.mean(xf * xf, axis=-1, keepdims=True) + EPS)
    return (y * g.astype(jnp.float32)).astype(x.dtype)


def swiglu(x, w_gu, w_down):
    gate, up = jnp.split(x @ w_gu, 2, axis=-1)
    return (jax.nn.silu(gate) * up) @ w_down


def t5_bucket(rel):
    n = jnp.maximum(rel, 0)
    max_exact = REL_BUCKETS // 2
    n_large = jnp.maximum(n, max_exact).astype(jnp.float32)
    large = max_exact + (jnp.log(n_large / max_exact) / math.log(REL_MAX_DIST / max_exact)
                         * (REL_BUCKETS - max_exact)).astype(jnp.int32)
    large = jnp.minimum(large, REL_BUCKETS - 1)
    return jnp.where(n < max_exact, n, large)


def causal_conv(x, w, b):
    c = x.shape[-1]
    y = lax.conv_general_dilated(x, w[:, None, :], window_strides=(1,),
                                 padding=((CONV_WIDTH - 1, 0),),
                                 dimension_numbers=('NWC', 'WIO', 'NWC'),
                                 feature_group_count=c)
    return y + b


def dsa_attention(q, c_kv, q_idx, k_idx, w_idx, rel_bias, topk):
    b, s = q.shape[:2]
    n_blocks = s // Q_BLOCK
    scale = KV_RANK ** -0.5
    s_pos = jnp.arange(s)

    def block(t0):
        qb = lax.dynamic_slice_in_dim(q, t0, Q_BLOCK, axis=1)
        qib = lax.dynamic_slice_in_dim(q_idx, t0, Q_BLOCK, axis=1)
        wib = lax.dynamic_slice_in_dim(w_idx, t0, Q_BLOCK, axis=1)
        t_pos = t0 + jnp.arange(Q_BLOCK)
        causal = s_pos[None, :] <= t_pos[:, None]
        dots = jnp.einsum('bthd,bsd->bths', qib, k_idx).astype(jnp.float32)
        score = jnp.einsum('bths,bth->bts', jax.nn.relu(dots), wib.astype(jnp.float32))
        score = jnp.where(causal[None], score, -jnp.inf)
        _, idx = lax.top_k(score, topk)
        valid = idx <= t_pos[None, :, None]
        kv_sel = jax.vmap(lambda c, i: c[i])(c_kv, idx)
        logits = jnp.einsum('bthr,btkr->bthk', qb, kv_sel).astype(jnp.float32) * scale
        bias = rel_bias[t5_bucket(t_pos[None, :, None] - idx)]
        logits = logits + jnp.transpose(bias, (0, 1, 3, 2)).astype(jnp.float32)
        logits = jnp.where(valid[:, :, None, :], logits, -jnp.inf)
        p = jax.nn.softmax(logits, axis=-1).astype(q.dtype)
        return jnp.einsum('bthk,btkr->bthr', p, kv_sel)

    out = lax.map(block, jnp.arange(n_blocks) * Q_BLOCK)
    return jnp.transpose(out, (1, 0, 2, 3, 4)).reshape(b, s, ATT_HEADS, KV_RANK)


def mlstm(q, k, v, i_pre, f_pre):
    out_dtype = v.dtype
    b, s = q.shape[:2]
    f32 = jnp.float32
    q = q.astype(f32)
    k = k.astype(f32) * (M_QK_DIM ** -0.5)
    v = v.astype(f32)
    ig = i_pre.astype(f32)
    lf = jax.nn.log_sigmoid(f_pre.astype(f32))

    def chunks(a):
        a = a.reshape((b, s // M_CHUNK, M_CHUNK) + a.shape[2:])
        return jnp.transpose(a, (1, 0, 3, 2) + tuple(range(4, a.ndim)))

    tril = jnp.tril(jnp.ones((M_CHUNK, M_CHUNK), dtype=bool))

    def step(carry, inp):
        c_st, n_st, m_st = carry
        qc, kc, vc, ic, fc = inp
        bcum = jnp.cumsum(fc, axis=-1)
        dmat = jnp.where(tril, bcum[..., :, None] - bcum[..., None, :] + ic[..., None, :], -jnp.inf)
        inter = bcum + m_st[..., None]
        m_row = jnp.maximum(inter, jnp.max(dmat, axis=-1))
        w_inter = jnp.exp(inter - m_row)
        qk = jnp.einsum('bhld,bhsd->bhls', qc, kc) * jnp.exp(dmat - m_row[..., None])
        num = (w_inter[..., None] * jnp.einsum('bhld,bhdv->bhlv', qc, c_st)
               + jnp.einsum('bhls,bhsv->bhlv', qk, vc))
        den = w_inter * jnp.einsum('bhld,bhd->bhl', qc, n_st) + jnp.sum(qk, axis=-1)
        h = num / jnp.maximum(jnp.abs(den), jnp.exp(-m_row))[..., None]
        b_last = bcum[..., -1]
        g = b_last[..., None] - bcum + ic
        m_new = jnp.maximum(b_last + m_st, jnp.max(g, axis=-1))
        decay = jnp.exp(b_last + m_st - m_new)
        wk = jnp.exp(g - m_new[..., None])
        c_new = decay[..., None, None] * c_st + jnp.einsum('bhs,bhsd,bhsv->bhdv', wk, kc, vc)
        n_new = decay[..., None] * n_st + jnp.einsum('bhs,bhsd->bhd', wk, kc)
        return (c_new, n_new, m_new), h

    init = (jnp.zeros((b, M_HEADS, M_QK_DIM, M_V_DIM), f32),
            jnp.zeros((b, M_HEADS, M_QK_DIM), f32),
            jnp.zeros((b, M_HEADS), f32))
    _, h = lax.scan(step, init, (chunks(q), chunks(k), chunks(v), chunks(ig), chunks(lf)))
    h = jnp.transpose(h, (1, 0, 3, 2, 4)).reshape(b, s, M_HEADS, M_V_DIM)
    return h.astype(out_dtype)


def setup_inputs(seed: int = 0) -> dict:
    key = jax.random.key(seed)
    ks = jax.random.split(key, 20)
    f32 = jnp.float32

    def nrm(k, shape, scale):
        return jax.random.normal(k, shape, f32) * scale

    def gain(k, shape):
        return 1.0 + 0.05 * jax.random.normal(k, shape, f32)

    L, D, F = DEPTH, D_MODEL, D_FF
    CQK = 2 * M_HEADS * M_QK_DIM
    return {
        'x': nrm(ks[0], (BATCH, SEQ, D), 1.0),
        'rel_bias': nrm(ks[1], (REL_BUCKETS, ATT_HEADS), 0.5),
        'ffn1_norm': gain(ks[2], (L, D)),
        'ffn1_w_gu': nrm(ks[3], (L, D, 2 * F), D ** -0.5),
        'ffn1_w_down': nrm(ks[4], (L, F, D), F ** -0.5),
        'mix_norm': gain(ks[5], (L, D)),
        'w_in': nrm(ks[6], (L, D, D_IN), D ** -0.5),
        'q_norm': gain(ks[7], (L, KV_RANK)),
        'kv_norm': gain(ks[8], (L, KV_RANK)),
        'conv_w': nrm(ks[9], (L, CONV_WIDTH, CQK), CONV_WIDTH ** -0.5),
        'conv_b': nrm(ks[10], (L, CQK), 0.02),
        'igate_b': nrm(ks[11], (L, M_HEADS), 0.1),
        'fgate_b': jnp.linspace(3.0, 6.0, M_HEADS, dtype=f32)[None, :] + nrm(ks[12], (L, M_HEADS), 0.1),
        'm_out_norm': gain(ks[13], (L, M_HEADS, M_V_DIM)),
        'w_att_out': nrm(ks[14], (L, ATT_HEADS, KV_RANK, D), (ATT_HEADS * KV_RANK) ** -0.5),
        'w_mem_out': nrm(ks[15], (L, M_HEADS * M_V_DIM, D), (M_HEADS * M_V_DIM) ** -0.5),
        'w_out': nrm(ks[16], (L, D, D), D ** -0.5),
        'ffn2_norm': gain(ks[17], (L, D)),
        'ffn2_w_gu': nrm(ks[18], (L, D, 2 * F), D ** -0.5),
        'ffn2_w_down': nrm(ks[19], (L, F, D), F ** -0.5),
    }


def reference(x, rel_bias, ffn1_norm, ffn1_w_gu, ffn1_w_down, mix_norm, w_in, q_norm, kv_norm,
              conv_w, conv_b, igate_b, fgate_b, m_out_norm, w_att_out, w_mem_out, w_out,
              ffn2_norm, ffn2_w_gu, ffn2_w_down):
    b, s, _ = x.shape
    topk = min(TOPK_MAX, s // 4)
    h = x
    for l in range(DEPTH):
        h = h + 0.5 * swiglu(rms_norm(h, ffn1_norm[l]), ffn1_w_gu[l], ffn1_w_down[l])
        u = rms_norm(h, mix_norm[l])
        proj = u @ w_in[l]
        (aq, ckv, iq, ik, iw, mqk, mv, mo, mi, mf, ga, gm) = jnp.split(proj, SPLIT_POINTS, axis=-1)
        aq = rms_norm(aq.reshape(b, s, ATT_HEADS, KV_RANK), q_norm[l])
        ckv = rms_norm(ckv, kv_norm[l])
        iq = iq.reshape(b, s, IDX_HEADS, IDX_DIM)
        iw = iw * (IDX_HEADS * IDX_DIM) ** -0.5
        o_att = dsa_attention(aq, ckv, iq, ik, iw, rel_bias, topk)
        y_att = jnp.einsum('bshr,hrd->bsd', o_att, w_att_out[l])
        mqk = jax.nn.silu(causal_conv(mqk, conv_w[l], conv_b[l]))
        mq, mk = jnp.split(mqk, 2, axis=-1)
        hm = mlstm(mq.reshape(b, s, M_HEADS, M_QK_DIM), mk.reshape(b, s, M_HEADS, M_QK_DIM),
                   mv.reshape(b, s, M_HEADS, M_V_DIM), mi + igate_b[l], mf + fgate_b[l])
        hm = rms_norm(hm, m_out_norm[l]).reshape(b, s, M_HEADS * M_V_DIM) * jax.nn.sigmoid(mo)
        y_mem = hm @ w_mem_out[l]
        merged = jax.nn.sigmoid(ga) * y_att + jax.nn.sigmoid(gm) * y_mem
        h = h + merged @ w_out[l]
        h = h + 0.5 * swiglu(rms_norm(h, ffn2_norm[l]), ffn2_w_gu[l], ffn2_w_down[l])
    return h
```

```python
import math
from contextlib import ExitStack
import numpy as np
import concourse.bass as bass
import concourse.mybir as mybir
from concourse.bass_utils import run_bass_kernel_spmd

F32 = mybir.dt.float32
BF16 = mybir.dt.bfloat16
AF = mybir.ActivationFunctionType
ALU = mybir.AluOpType
AX = mybir.AxisListType

D = 2048
S = 2048
NL = 4
FF = 5504
NFC = FF // 128
H = 8
R = 256
HI = 8
DI = 64
TOPK = 256
MH = 4
DK = 128
DV = 256
EPS = 1e-6
D_IN = 10064
OFF_AQ, OFF_CKV, OFF_IQ, OFF_IK, OFF_IW, OFF_MQK, OFF_MV, OFF_MO, OFF_MI, OFF_MF, OFF_GA, OFF_GM = (
    0, 2048, 2304, 2816, 2880, 2888, 3912, 4936, 5960, 5964, 5968, 8016)
NEG = -30000.0

C_FFN1, C_MIX, C_FFN2 = 0, 16, 32
C_QN, C_KVN = 48, 50
C_CONVW = 52
C_CONVB = 84
C_MON = 92
C_IB, C_FB = 100, 101
NCOLS = 104


class Buf:
    __slots__ = ("name", "w", "r", "multi", "dsem", "dcnt")

    def __init__(self, name, multi=False):
        self.name = name
        self.w = {}
        self.r = {}
        self.multi = multi
        self.dsem = None
        self.dcnt = 0


class Queue:
    def __init__(self, name, sem):
        self.name = name
        self.sem = sem
        self.cnt = 0
        self.known = {}
        self.ops = []


class Ctx:
    def __init__(self, nc, es):
        self.nc = nc
        self.es = es
        self.q = {}
        for n in ("pe", "act", "dve", "pool", "sp"):
            self.q[n] = Queue(n, es.enter_context(nc.semaphore("q_" + n)))
        self.bufs = []
        self.dma_sems = []
        self.sem_pool = []
        self.scopes = []
        self.ninstr = 0
        self.nsem = 0

    def buf(self, name, multi=False):
        b = Buf(name, multi)
        self.bufs.append(b)
        if self.scopes:
            self.scopes[-1].append(b)
        return b

    def push_scope(self):
        self.scopes.append([])

    def pop_scope(self):
        for b in self.scopes.pop():
            self.bufs.remove(b)
            if b.dsem is not None:
                self.dma_sems.remove(b)
                self.sem_pool.append((b.dsem, b.dcnt))

    def _dsem(self, b):
        if b.dsem is None:
            if self.sem_pool:
                b.dsem, b.dcnt = self.sem_pool.pop()
            else:
                self.nsem += 1
                b.dsem = self.es.enter_context(self.nc.semaphore("d%d" % self.nsem))
            self.dma_sems.append(b)
        return b.dsem

    def op(self, qn, fn, reads=(), writes=(), dma=None, inc=True):
        q = self.q[qn]
        deps = {}
        own = self._dsem(dma) if dma is not None else None

        def merge(d, same_ok):
            for s, v in d.items():
                if s is own:
                    continue
                if (s is q.sem) and dma is None:
                    if qn == "pe" or not same_ok:
                        continue
                if deps.get(s, 0) < v:
                    deps[s] = v
        for b in reads:
            merge(b.w, True)
        for b in writes:
            if not b.multi:
                merge(b.w, False)
            merge(b.r, False)
        waits = []
        for s, v in deps.items():
            if q.known.get(s, 0) >= v:
                continue
            q.known[s] = v
            waits.append((s, v))
        if dma is not None:
            sem = self._dsem(dma)
            dma.dcnt += 16
            val = dma.dcnt
            assert val < 65000, ("dma sem overflow", dma.name)
            q.ops.append((waits, fn, sem, 16))
        else:
            sem = q.sem
            if inc:
                q.cnt += 1
                val = q.cnt
                assert val < 65000, ("sem overflow", qn)
                q.ops.append((waits, fn, sem, 1))
            else:
                val = q.cnt + 1
                q.ops.append((waits, fn, None, 0))
        self.ninstr += 1 + len(waits)
        for b in reads:
            if b.r.get(sem, 0) < val:
                b.r[sem] = val
        for b in writes:
            if b.multi:
                if b.w.get(sem, 0) < val:
                    b.w[sem] = val
            else:
                b.w = {sem: val}
                b.r = {}

    def dma(self, qn, out, in_, reads, writes, dma):
        self.op(qn, lambda e: e.dma_start(out=out, in_=in_), reads=reads, writes=writes, dma=dma)

    def act(self, out, in_, func, reads, writes, bias=None, scale=1.0, accum_out=None):
        kw = {}
        if bias is not None:
            kw["bias"] = bias
        if accum_out is not None:
            kw["accum_out"] = accum_out
        self.op("act", lambda e: e.activation(out=out, in_=in_, func=func, scale=scale, **kw), reads=reads, writes=writes)

    def mm(self, out, lhsT, rhs, start, stop, reads, writes, inc=True):
        self.op("pe", lambda e: e.matmul(out, lhsT=lhsT, rhs=rhs, start=start, stop=stop), reads=reads, writes=writes, inc=inc)

    def tr(self, out, in_, ident, reads, writes, inc=True):
        self.op("pe", lambda e: e.transpose(out, in_, ident), reads=reads, writes=writes, inc=inc)

    def tt(self, qn, out, in0, in1, op, reads, writes):
        self.op(qn, lambda e: e.tensor_tensor(out=out, in0=in0, in1=in1, op=op), reads=reads, writes=writes)

    def stt(self, out, in0, scalar, in1, op0, op1, reads, writes):
        self.op("dve", lambda e: e.scalar_tensor_tensor(out=out, in0=in0, scalar=scalar, in1=in1, op0=op0, op1=op1),
                reads=reads, writes=writes)

    def ts(self, qn, out, in0, s1, s2, op0, op1, reads, writes):
        if s2 is None:
            self.op(qn, lambda e: e.tensor_scalar(out=out, in0=in0, scalar1=s1, scalar2=None, op0=op0), reads=reads, writes=writes)
        else:
            self.op(qn, lambda e: e.tensor_scalar(out=out, in0=in0, scalar1=s1, scalar2=s2, op0=op0, op1=op1),
                    reads=reads, writes=writes)

    def copy(self, qn, out, in_, reads, writes):
        if qn == "act":
            self.op(qn, lambda e: e.activation(out=out, in_=in_, func=AF.Copy), reads=reads, writes=writes)
        else:
            self.op(qn, lambda e: e.tensor_copy(out=out, in_=in_), reads=reads, writes=writes)

    def recip(self, out, in_, reads, writes):
        self.op("dve", lambda e: e.reciprocal(out=out, in_=in_), reads=reads, writes=writes)

    def memset(self, qn, out, val, writes):
        self.op(qn, lambda e: e.memset(out, val), reads=[], writes=writes)

    def barrier(self):
        toks = {}
        for qn, q in self.q.items():
            if q.cnt > 0:
                toks[q.sem] = q.cnt
        for b in self.dma_sems:
            if b.dcnt > 0:
                toks[b.dsem] = b.dcnt
        for qn, q in self.q.items():
            waits = []
            for s, v in toks.items():
                if s is q.sem and qn == "pe":
                    continue
                if q.known.get(s, 0) >= v:
                    continue
                q.known[s] = v
                waits.append((s, v))
            if waits:
                q.ops.append((waits, None, None, 0))
                self.ninstr += len(waits)
        for b in self.bufs:
            b.w = {}
            b.r = {}

    def emit(self):
        nc = self.nc
        with nc.Block() as block:
            def run(q):
                def f(e):
                    for waits, fn, sem, inc in q.ops:
                        for s, v in waits:
                            e.wait_ge(s, v)
                        if fn is not None:
                            ins = fn(e)
                            if sem is not None:
                                ins.then_inc(sem, inc)
                return f
            block.tensor(run(self.q["pe"]))
            block.scalar(run(self.q["act"]))
            block.vector(run(self.q["dve"]))
            block.gpsimd(run(self.q["pool"]))
            block.sync(run(self.q["sp"]))


class Arena:
    def __init__(self, nc, es, nbytes):
        self.t = es.enter_context(nc.sbuf_tensor("arena", [128, nbytes // 4], F32))
        self.nbytes = nbytes
        self.top = 0

    def mark(self):
        return self.top

    def reset(self, m):
        self.top = m

    def alloc(self, shape, dtype):
        esz = 4 if dtype == F32 else 2
        n = 1
        for s in shape:
            n *= s
        nb = (n * esz + 31) // 32 * 32
        assert self.top + nb <= self.nbytes, ("arena overflow", self.top, nb, self.nbytes)
        o = self.top // 4
        ap = self.t[:, o:o + nb // 4]
        self.top += nb
        if dtype != F32:
            ap = ap.bitcast(dtype)
        ap = ap[:, 0:n]
        if len(shape) == 2:
            ap = ap.rearrange("p (a b) -> p a b", a=shape[0])
        elif len(shape) == 3:
            ap = ap.rearrange("p (a b c) -> p a b c", a=shape[0], b=shape[1])
        return ap


class K:
    dbg_on = False

    def dump(self, name, ap, bufs, dtype=F32):
        if not self.dbg_on or True:
            return
        shp = list(ap.shape)
        d = self.nc.dram_tensor("dbg_" + name, shp, dtype, kind="ExternalOutput").ap()
        b = self.c.buf("dbg_" + name)
        self.c.dma("sp", d, ap, bufs, [], b)


def ffn_stage(k, src, dst, gcol, wgu, wd):
    c, ar, nc = k.c, k.ar, k.nc
    NT = 1024
    m0 = ar.mark()
    c.push_scope()
    actT = ar.alloc([NFC, NT], BF16)
    hk = ar.t[:, m0 // 4:m0 // 4 + 16 * NT].rearrange("p (a b) -> p a b", a=16)
    b_hk = [c.buf("hk%d" % i) for i in range(4)]
    wgt = [ar.alloc([16, 128], BF16) for _ in range(3)]
    wut = [ar.alloc([16, 128], BF16) for _ in range(3)]
    regU = ar.mark()
    uT = ar.alloc([16, NT], BF16)
    regX = ar.mark()
    hld = [ar.alloc([NT], F32) for _ in range(2)]
    sqt = [ar.alloc([NT], F32) for _ in range(2)]
    rstd = ar.alloc([NT], F32)
    tmp = ar.alloc([NT], F32)
    ar.reset(regX)
    sgt = [ar.alloc([NT], F32) for _ in range(2)]
    ar.reset(regU)
    wdt = [ar.alloc([NFC, 128], BF16) for _ in range(2)]
    hres = [ar.alloc([NT], F32) for _ in range(2)]
    ar.top = max(ar.top, regX + 6 * NT * 4)

    b_act = [c.buf("act%d" % i) for i in range(NFC)]
    b_wg = [c.buf("wg%d" % i) for i in range(3)]
    b_wu = [c.buf("wu%d" % i) for i in range(3)]
    b_uT = [c.buf("uT%d" % i) for i in range(16)]
    b_hld = [c.buf("hld%d" % i) for i in range(2)]
    b_sqt = [c.buf("sqt%d" % i) for i in range(2)]
    b_rstd = c.buf("rstd")
    b_tmp = c.buf("tmp")
    b_sgt = [c.buf("sgt%d" % i) for i in range(2)]
    b_wd = [c.buf("wd%d" % i) for i in range(2)]
    b_hres = [c.buf("hres%d" % i) for i in range(2)]
    b_hst = [c.buf("hst%d" % i) for i in range(2)]
    ps, b_ps = k.ps, k.b_ps
    wgu_v = wgu.rearrange("(kc p) m -> p kc m", p=128)
    wd_v = wd.rearrange("(fc p) m -> p fc m", p=128)

    for tb in range(S // NT):
        t0 = tb * NT
        for cc in range(16):
            c.dma("sp", hk[:, cc, :], src[cc * 128:(cc + 1) * 128, t0:t0 + NT], [k.b_h], [b_hk[cc // 4]], b_hk[cc // 4])
        for cc in range(16):
            sl = cc % 2
            c.act(sqt[sl], hk[:, cc, :], AF.Square, [b_hk[cc // 4]], [b_sqt[sl]])
            for hf in range(2):
                c.mm(ps[hf], k.ones_f, sqt[sl][:, hf * 512:(hf + 1) * 512], cc == 0, cc == 15,
                     [b_sqt[sl]], [b_ps[hf]], inc=(hf == 1))
        for hf in range(2):
            c.act(tmp[:, hf * 512:(hf + 1) * 512], ps[hf], AF.Ln, [b_ps[hf]], [b_tmp], bias=k.eps_col, scale=1.0 / D)
        c.act(rstd, tmp, AF.Exp, [b_tmp], [b_rstd], scale=-0.5)
        for cc in range(16):
            c.stt(uT[:, cc, :], hk[:, cc, :], gcol[:, cc:cc + 1], rstd, ALU.mult, ALU.mult, [b_hk[cc // 4], b_rstd], [b_uT[cc]])
        if tb == 0:
            k.dump("rstd", rstd, [b_rstd])
            k.dump("uT0", uT[:, 0, :], [b_uT[0]], BF16)
            k.dump("uT5", uT[:, 5, :], [b_uT[5]], BF16)
        c.barrier()
        for f in range(NFC):
            sl = f % 3
            pset = (f % 2) * 4
            c.dma("pool", wgt[sl], wgu_v[:, :, f * 128:(f + 1) * 128], [], [b_wg[sl]], b_wg[sl])
            c.dma("pool", wut[sl], wgu_v[:, :, FF + f * 128:FF + (f + 1) * 128], [], [b_wu[sl]], b_wu[sl])
            for wi, (wt, bw) in enumerate(((wgt, b_wg), (wut, b_wu))):
                for hf in range(2):
                    bank = pset + wi * 2 + hf
                    for kc in range(16):
                        c.mm(ps[bank], wt[sl][:, kc, :], uT[:, kc, hf * 512:(hf + 1) * 512], kc == 0, kc == 15,
                             [bw[sl], b_uT[kc]], [b_ps[bank]], inc=(kc == 15))
            ss = f % 2
            for hf in range(2):
                c.act(sgt[ss][:, hf * 512:(hf + 1) * 512], ps[pset + hf], AF.Silu, [b_ps[pset + hf]], [b_sgt[ss]])
            for hf in range(2):
                c.tt("dve", actT[:, f, hf * 512:(hf + 1) * 512], sgt[ss][:, hf * 512:(hf + 1) * 512], ps[pset + 2 + hf], ALU.mult,
                     [b_sgt[ss], b_ps[pset + 2 + hf]], [b_act[f]])
        if tb == 0:
            k.dump("act0", actT[:, 0, :], [b_act[0]], BF16)
            k.dump("act7", actT[:, 7, :], [b_act[7]], BF16)
        c.barrier()
        for dc in range(16):
            sl = dc % 2
            for g0 in range(0, NFC, 11):
                g1 = min(NFC, g0 + 11)
                c.dma("pool", wdt[sl][:, g0:g1, :], wd_v[:, g0:g1, dc * 128:(dc + 1) * 128], [], [b_wd[sl]], b_wd[sl])
            c.dma("sp", hres[sl], src[dc * 128:(dc + 1) * 128, t0:t0 + NT], [k.b_h], [b_hres[sl]], b_hres[sl])
            pset = (dc % 4) * 2
            for hf in range(2):
                for fc in range(NFC):
                    c.mm(ps[pset + hf], wdt[sl][:, fc, :], actT[:, fc, hf * 512:(hf + 1) * 512], fc == 0, fc == NFC - 1,
                         [b_wd[sl], b_act[fc]], [b_ps[pset + hf]], inc=(fc == NFC - 1))
            for hf in range(2):
                c.stt(hres[sl][:, hf * 512:(hf + 1) * 512], ps[pset + hf], 0.5, hres[sl][:, hf * 512:(hf + 1) * 512],
                      ALU.mult, ALU.add, [b_ps[pset + hf], b_hres[sl]], [b_hres[sl]])
            c.dma("sp", dst[dc * 128:(dc + 1) * 128, t0:t0 + NT], hres[sl], [b_hres[sl]], [k.b_h], b_hst[sl])
        c.barrier()
    c.pop_scope()
    ar.reset(m0)


def build(plan, n_layers=NL):
    nc = bass.Bass("TRN2", target_bir_lowering=False)
    k = K()
    k.nc = nc
    dt = nc.dram_tensor
    xT = dt("xT", [D, S], F32, kind="ExternalInput").ap()
    cols_d = dt("cols", [128, NL * NCOLS], F32, kind="ExternalInput").ap()
    cst_d = dt("cst", [128, 3 * 128], F32, kind="ExternalInput").ap()
    tbraw_d = dt("tbraw", [128, 2 * 8 * 128], F32, kind="ExternalInput").ap()
    rb31_d = dt("rb31", [128, 8], F32, kind="ExternalInput").ap()
    k.w = {}
    k.w["ffn1_w_gu"] = dt("ffn1_w_gu", [NL, D, 2 * FF], F32, kind="ExternalInput").ap()
    k.w["ffn1_w_down"] = dt("ffn1_w_down", [NL, FF, D], F32, kind="ExternalInput").ap()
    k.w["w_in"] = dt("w_in", [NL, D, D_IN], F32, kind="ExternalInput").ap()
    k.w["w_att_out"] = dt("w_att_out", [NL, H * R, D], F32, kind="ExternalInput").ap()
    k.w["w_mem_out"] = dt("w_mem_out", [NL, MH * DV, D], F32, kind="ExternalInput").ap()
    k.w["w_out"] = dt("w_out", [NL, D, D], F32, kind="ExternalInput").ap()
    k.w["ffn2_w_gu"] = dt("ffn2_w_gu", [NL, D, 2 * FF], F32, kind="ExternalInput").ap()
    k.w["ffn2_w_down"] = dt("ffn2_w_down", [NL, FF, D], F32, kind="ExternalInput").ap()
    outT = dt("outT", [D, S], F32, kind="ExternalOutput").ap()
    k.xT, k.outT = xT, outT
    ikind = "ExternalOutput" if K.dbg_on else "Internal"
    k.hbuf = dt("hbuf", [D, S], F32, kind=ikind).ap()
    k.dbg = {}

    with ExitStack() as es:
        c = Ctx(nc, es)
        k.c = c
        ar = Arena(nc, es, 207 * 1024)
        k.ar = ar
        k.ps = []
        k.b_ps = []
        for i in range(8):
            t = es.enter_context(nc.psum_tensor("ps%d" % i, [128, 512], F32))
            k.ps.append(t[:, :])
            k.b_ps.append(c.buf("ps%d" % i))
        k.b_h = c.buf("hdram", multi=True)
        k.cols = ar.alloc([NL * NCOLS], F32)
        k.cst = ar.alloc([3 * 128], F32)
        k.ones_f = ar.alloc([128], F32)
        k.ones_b = ar.alloc([128], BF16)
        k.eps_col = ar.alloc([1], F32)
        k.ident_b = ar.alloc([128], BF16)
        k.ident4_b = ar.alloc([4, 128], BF16)
        k.tri_b = ar.alloc([128], BF16)
        k.ident_f = k.cst[:, 0:128]
        k.cneg_f = k.cst[:, 128:256]
        k.b_const = c.buf("const")
        b_ld = c.buf("cld")
        c.dma("sp", k.cols, cols_d, [], [k.b_const], b_ld)
        c.dma("sp", k.cst, cst_d, [], [k.b_const], b_ld)
        c.memset("dve", k.ones_f, 1.0, [k.b_const])
        c.memset("dve", k.ones_b, 1.0, [k.b_const])
        c.memset("dve", k.eps_col, EPS, [k.b_const])
        c.barrier()
        c.copy("dve", k.ident_b, k.cst[:, 0:128], [], [k.b_const])
        c.copy("dve", k.tri_b, k.cst[:, 256:384], [], [k.b_const])
        for i in range(4):
            c.copy("dve", k.ident4_b[:, i, :], k.cst[:, 0:128], [], [k.b_const])
        c.barrier()

        def col(l, cidx, n=1):
            return k.cols[:, l * NCOLS + cidx: l * NCOLS + cidx + n]
        k.col = col

        k.one_col = ar.alloc([1], F32)
        c.memset("dve", k.one_col, 1.0, [k.b_const])
        k.TB = ar.alloc([2, 8, 128], F32)
        k.b_TB = c.buf("TB")
        rb31 = ar.alloc([8], F32)
        c.dma("sp", k.TB.rearrange("p a b c -> p (a b c)"), tbraw_d, [], [k.b_TB], b_ld)
        c.dma("sp", rb31, rb31_d, [], [k.b_const], b_ld)
        for kind in range(2):
            for h in range(8):
                c.ts("dve", k.TB[:, kind, h, :], k.TB[:, kind, h, :], rb31[:, h:h + 1], None, ALU.subtract, None,
                     [k.b_TB, k.b_const], [k.b_TB])
        k.TBh = ar.alloc([2, 8, 128], BF16)
        k.TBl = ar.alloc([2, 8, 128], BF16)
        tbf = k.TB.rearrange("p a b c -> p (a b c)")
        c.copy("dve", k.TBh.rearrange("p a b c -> p (a b c)"), tbf, [k.b_TB], [k.b_TB])
        c.tt("dve", k.TBl.rearrange("p a b c -> p (a b c)"), tbf, k.TBh.rearrange("p a b c -> p (a b c)"), ALU.subtract,
             [k.b_TB], [k.b_TB])
        c.barrier()
        k.b_scr = c.buf("scr", multi=True)
        k.q_scr = dt("q_scr", [16, 128, 2, 8, 128], BF16, kind=ikind).ap()
        k.o_scr = dt("o_scr", [16, 128, S], BF16, kind=ikind).ap()
        k.mqk_scr = dt("mqk_scr", [8, 128, S], BF16, kind=ikind).ap()
        k.v_scr = dt("v_scr", [4, 128, 16, 256], BF16, kind=ikind).ap()
        k.sgo_scr = dt("sgo_scr", [8, 128, S], BF16, kind=ikind).ap()
        k.sga_scr = dt("sga_scr", [16, 128, S], BF16, kind=ikind).ap()
        k.sgm_scr = dt("sgm_scr", [16, 128, S], BF16, kind=ikind).ap()
        k.hm_scr = dt("hm_scr", [8, 128, S], BF16, kind=ikind).ap()
        mL = ar.mark()

        def alloc_mixer():
            ar.reset(mL)
            k.gi = ar.alloc([S], F32)
            k.gf = ar.alloc([S], F32)
            k.b_gi, k.b_gf = c.buf("gi"), c.buf("gf")
            k.mA = ar.mark()
            k.ckvT = ar.alloc([2, S], BF16)
            k.ckv = ar.alloc([16, 256], BF16)
            k.iqT = ar.alloc([4, S], BF16)
            k.ikT = ar.alloc([S], BF16)
            k.iw = ar.alloc([16, 8], F32)
            k.b_ckvT, k.b_ckv, k.b_iqT, k.b_ikT, k.b_iw = (c.buf(n) for n in ("ckvT", "ckv", "iqT", "ikT", "iw"))

        for st in plan:
            if st[0] == "m1":
                alloc_mixer()
                m1_stage(k, st[1])
            elif st[0] == "m2":
                m2_stage(k, st[1])
                ar.reset(k.mA)
            elif st[0] == "m3":
                m3_stage(k, st[1])
                ar.reset(mL)
            elif st[0] == "m4":
                m4_stage(k, st[1])
            elif st[0] == "copy":
                pass
            elif st[0] == "ffn":
                _, l, which, src, dst = st
                srcap = {"x": xT, "h": k.hbuf, "o": outT}[src]
                dstap = {"x": xT, "h": k.hbuf, "o": outT}[dst]
                wn = "ffn1" if which == 1 else "ffn2"
                ffn_stage(k, srcap, dstap, col(l, C_FFN1 if which == 1 else C_FFN2, 16),
                          k.w[wn + "_w_gu"][l], k.w[wn + "_w_down"][l])
            else:
                raise ValueError(st)
        c.barrier()
        c.emit()
    print("instructions ~", c.ninstr, "dma sems", c.nsem)
    return nc


def host_consts(rel_bias):
    ident = np.eye(128, dtype=np.float32)
    tl = np.arange(128)
    cneg = np.where(tl[None, :] <= tl[:, None], 0.0, -1e30).astype(np.float32)
    tri = (tl[None, :] >= tl[:, None]).astype(np.float32)
    cst = np.concatenate([ident, cneg, tri], axis=1)
    d = np.arange(256)
    n = np.maximum(d, 0)
    nl = np.maximum(n, 16).astype(np.float32)
    large = 16 + (np.log(nl / 16) / math.log(128 / 16) * 16).astype(np.int32)
    large = np.minimum(large, 31)
    bucket = np.where(n < 16, n, large)
    sl = np.arange(128)[:, None]
    tt = np.arange(128)[None, :]
    tb = np.zeros((128, 2, 8, 128), np.float32)
    for kind in range(2):
        dd = np.clip(tt - sl + 128 * kind, 0, 255)
        tb[:, kind, :, :] = np.transpose(rel_bias[bucket[dd]], (0, 2, 1))
    rb31 = np.broadcast_to(rel_bias[31][None, :], (128, 8)).copy()
    return cst, tb.reshape(128, -1), rb31


def host_cols(inp):
    cols = np.zeros((NL, 128, NCOLS), np.float32)
    for l in range(NL):
        cols[l, :, C_FFN1:C_FFN1 + 16] = inp["ffn1_norm"][l].reshape(16, 128).T
        cols[l, :, C_MIX:C_MIX + 16] = inp["mix_norm"][l].reshape(16, 128).T
        cols[l, :, C_FFN2:C_FFN2 + 16] = inp["ffn2_norm"][l].reshape(16, 128).T
        cols[l, :, C_QN:C_QN + 2] = inp["q_norm"][l].reshape(2, 128).T
        cols[l, :, C_KVN:C_KVN + 2] = inp["kv_norm"][l].reshape(2, 128).T
        cols[l, :, C_CONVW:C_CONVW + 32] = inp["conv_w"][l].reshape(4, 8, 128).transpose(2, 0, 1).reshape(128, 32)
        cols[l, :, C_CONVB:C_CONVB + 8] = inp["conv_b"][l].reshape(8, 128).T
        cols[l, :, C_MON:C_MON + 8] = inp["m_out_norm"][l].reshape(4, 2, 128).transpose(2, 0, 1).reshape(128, 8)
        cols[l, 0:4, C_IB] = inp["igate_b"][l]
        cols[l, 0:4, C_FB] = inp["fgate_b"][l]
    return np.ascontiguousarray(cols.transpose(1, 0, 2).reshape(128, NL * NCOLS))


def rms_block(k, src, t0, NT, gcol, uT, b_uT):
    c, ar = k.c, k.ar
    ps, b_ps = k.ps, k.b_ps
    m0 = ar.mark()
    c.push_scope()
    hld = [ar.alloc([NT], F32) for _ in range(2)]
    sqt = [ar.alloc([NT], F32) for _ in range(2)]
    rstd = ar.alloc([NT], F32)
    tmp = ar.alloc([NT], F32)
    b_hld = [c.buf("hld%d" % i) for i in range(2)]
    b_sqt = [c.buf("sqt%d" % i) for i in range(2)]
    b_rstd = c.buf("rstd")
    b_tmp = c.buf("tmp")
    nh = NT // 512
    for cc in range(16):
        sl = cc % 2
        c.dma("sp", hld[sl], src[cc * 128:(cc + 1) * 128, t0:t0 + NT], [k.b_h], [b_hld[sl]], b_hld[sl])
        c.act(sqt[sl], hld[sl], AF.Square, [b_hld[sl]], [b_sqt[sl]])
        for hf in range(nh):
            c.mm(ps[hf], k.ones_f, sqt[sl][:, hf * 512:(hf + 1) * 512], cc == 0, cc == 15,
                 [b_sqt[sl]], [b_ps[hf]], inc=(hf == nh - 1))
    for hf in range(nh):
        c.act(tmp[:, hf * 512:(hf + 1) * 512], ps[hf], AF.Ln, [b_ps[hf]], [b_tmp], bias=k.eps_col, scale=1.0 / D)
    c.act(rstd, tmp, AF.Exp, [b_tmp], [b_rstd], scale=-0.5)
    for cc in range(16):
        sl = cc % 2
        c.dma("sp", hld[sl], src[cc * 128:(cc + 1) * 128, t0:t0 + NT], [k.b_h], [b_hld[sl]], b_hld[sl])
        c.stt(uT[:, cc, :], hld[sl], gcol[:, cc:cc + 1], rstd, ALU.mult, ALU.mult, [b_hld[sl], b_rstd], [b_uT[cc]])
    c.barrier()
    c.pop_scope()
    ar.reset(m0)


def m1_stage(k, l):
    c, ar = k.c, k.ar
    ps, b_ps = k.ps, k.b_ps
    NT = 1024
    src = k.hbuf
    wv = k.w["w_in"][l].rearrange("(kc p) m -> p kc m", p=128)
    m0 = ar.mark()
    c.push_scope()
    uT = ar.alloc([16, NT], BF16)
    b_uT = [c.buf("uT%d" % i) for i in range(16)]
    wt = [ar.alloc([16, 128], BF16) for _ in range(3)]
    b_wt = [c.buf("wt%d" % i) for i in range(3)]
    raw = [ar.alloc([NT], F32) for _ in range(2)]
    b_raw = [c.buf("raw%d" % i) for i in range(2)]
    sqq = [ar.alloc([NT], BF16) for _ in range(2)]
    b_sqq = [c.buf("sqq%d" % i) for i in range(2)]
    tmpn = ar.alloc([NT], F32)
    b_tmpn = c.buf("tmpn")
    rinv = ar.alloc([NT], F32)
    b_rinv = c.buf("rinv")
    stg = [ar.alloc([NT], BF16) for _ in range(3)]
    b_stg = [c.buf("stg%d" % i) for i in range(3)]
    b_sst = [c.buf("sst%d" % i) for i in range(3)]
    rawc = [ar.alloc([NT + 8], F32) for _ in range(2)]
    b_rawc = [c.buf("rawc%d" % i) for i in range(2)]
    acc = [ar.alloc([NT], F32) for _ in range(2)]
    b_acc = [c.buf("acc%d" % i) for i in range(2)]
    halo = ar.alloc([8, 4], F32)
    b_halo = c.buf("halo")
    iwrow = ar.alloc([NT], F32)
    b_iwrow = c.buf("iwrow")
    vst = [ar.alloc([8, 128], BF16) for _ in range(2)]
    b_vst = [c.buf("vst%d" % i) for i in range(2)]
    b_vss = [c.buf("vss%d" % i) for i in range(2)]
    gqs = ar.alloc([2], F32)
    b_gqs = c.buf("gqs")
    c.ts("dve", gqs, k.col(l, C_QN, 2), R ** -0.5, None, ALU.mult, None, [k.b_const], [b_gqs])
    ps6b = ps[6].bitcast(BF16)
    st = {"w": 0, "p": 0, "s": 0}

    def proj(col0, width, dup=False):
        sl = st["w"] % 3
        st["w"] += 1
        bank0 = (st["p"] % 2) * 2
        st["p"] += 1
        if dup:
            c.dma("pool", wt[sl][:, :, 0:64], wv[:, :, col0:col0 + 64], [], [b_wt[sl]], b_wt[sl])
            c.dma("pool", wt[sl][:, :, 64:128], wv[:, :, col0:col0 + 64], [], [b_wt[sl]], b_wt[sl])
            width = 128
        else:
            c.dma("pool", wt[sl][:, :, 0:width], wv[:, :, col0:col0 + width], [], [b_wt[sl]], b_wt[sl])
        for hf in range(2):
            for kc in range(16):
                c.mm(ps[bank0 + hf][0:width, :], wt[sl][:, kc, 0:width], uT[:, kc, hf * 512:(hf + 1) * 512], kc == 0, kc == 15,
                     [b_wt[sl], b_uT[kc]], [b_ps[bank0 + hf]], inc=(kc == 15))
        return bank0

    def stage_slot():
        s_ = st["s"] % 3
        st["s"] += 1
        return s_

    import os
    SEC = os.environ.get("M1_SEC", "q,iq,ik,iw,mqk,mv,gates,gi").split(",")
    for th in range(2):
        t0 = th * NT
        rms_block(k, src, t0, NT, k.col(l, C_MIX, 16), uT, b_uT)
        for g in (range(9) if "q" in SEC else []):
            gcols = gqs if g < 8 else k.col(l, C_KVN, 2)
            b_gc = b_gqs if g < 8 else k.b_const
            for rc in range(2):
                bank0 = proj(OFF_AQ + g * 256 + rc * 128, 128)
                for hf in range(2):
                    c.copy("dve", raw[rc][:, hf * 512:(hf + 1) * 512], ps[bank0 + hf], [b_ps[bank0 + hf]], [b_raw[rc]])
                    if not os.environ.get("NOSQ"):
                        c.act(sqq[rc][:, hf * 512:(hf + 1) * 512], raw[rc][:, hf * 512:(hf + 1) * 512], AF.Square, [b_raw[rc]], [b_sqq[rc]])
                for hf in range(2):
                    if not os.environ.get("NOSSQ"):
                        c.mm(ps[4 + hf], k.ones_b, sqq[rc][:, hf * 512:(hf + 1) * 512], rc == 0, rc == 1,
                             [b_sqq[rc]], [b_ps[4 + hf]], inc=True)
            for hf in range(2):
                c.act(tmpn[:, hf * 512:(hf + 1) * 512], ps[4 + hf], AF.Ln, [b_ps[4 + hf]], [b_tmpn], bias=k.eps_col, scale=1.0 / R)
            c.act(rinv, tmpn, AF.Exp, [b_tmpn], [b_rinv], scale=-0.5)
            for rc in range(2):
                if g < 8:
                    ss = stage_slot()
                    c.stt(stg[ss], raw[rc], gcols[:, rc:rc + 1], rinv, ALU.mult, ALU.mult, [b_raw[rc], b_rinv, b_gc], [b_stg[ss]])
                    dstq = k.q_scr[th * 8:(th + 1) * 8, :, rc, g, :].rearrange("j p t -> p j t")
                    if not os.environ.get("NOQSTORE"):
                        c.dma("sp", dstq, stg[ss].rearrange("p (j t) -> p j t", j=8), [b_stg[ss]], [k.b_scr], b_sst[ss])
                else:
                    c.stt(k.ckvT[:, rc, t0:t0 + NT], raw[rc], gcols[:, rc:rc + 1], rinv, ALU.mult, ALU.mult,
                          [b_raw[rc], b_rinv, b_gc], [k.b_ckvT])
            if g == 8 and not os.environ.get("NOCKVTR"):
                for grp in range(2):
                    for tl in range(4):
                        tile_ = th * 8 + grp * 4 + tl
                        for rc in range(2):
                            c.tr(ps6b[:, (tl * 2 + rc) * 128:(tl * 2 + rc + 1) * 128], k.ckvT[:, rc, tile_ * 128:(tile_ + 1) * 128],
                                 k.ident_b, [k.b_ckvT, k.b_const], [b_ps[6]], inc=(tl == 3 and rc == 1))
                    c.copy("dve", k.ckv[:, th * 8 + grp * 4:th * 8 + grp * 4 + 4, :].rearrange("p a b -> p (a b)"), ps6b,
                           [b_ps[6]], [k.b_ckv])
        for cc in (range(4) if "iq" in SEC else []):
            bank0 = proj(OFF_IQ + cc * 128, 128)
            for hf in range(2):
                c.copy("act", k.iqT[:, cc, t0 + hf * 512:t0 + (hf + 1) * 512], ps[bank0 + hf], [b_ps[bank0 + hf]], [k.b_iqT])
        if "ik" in SEC:
            bank0 = proj(OFF_IK, 64, dup=True)
            for hf in range(2):
                c.copy("act", k.ikT[:, t0 + hf * 512:t0 + (hf + 1) * 512], ps[bank0 + hf], [b_ps[bank0 + hf]], [k.b_ikT])
        bank0 = proj(OFF_IW, 8) if "iw" in SEC else 0
        for hf in (range(2) if "iw" in SEC else []):
            c.act(iwrow[0:8, hf * 512:(hf + 1) * 512], ps[bank0 + hf][0:8, :], AF.Copy, [b_ps[bank0 + hf]], [b_iwrow],
                  scale=float((HI * DI) ** -0.5))
        for tl in (range(8) if "iw" in SEC else []):
            c.tr(ps[7][:, tl * 8:(tl + 1) * 8], iwrow[0:8, tl * 128:(tl + 1) * 128], k.ident_f[0:8, 0:8],
                 [b_iwrow, k.b_const], [b_ps[7]], inc=(tl == 7))
        if "iw" in SEC:
            c.copy("dve", k.iw[:, th * 8:(th + 1) * 8, :].rearrange("p a b -> p (a b)"), ps[7][:, 0:64], [b_ps[7]], [k.b_iw])
        for cc in (range(8) if "mqk" in SEC else []):
            bank0 = proj(OFF_MQK + cc * 128, 128)
            rs = cc % 2
            for hf in range(2):
                c.copy("act", rawc[rs][:, 3 + hf * 512:3 + (hf + 1) * 512], ps[bank0 + hf], [b_ps[bank0 + hf]], [b_rawc[rs]])
            if th == 0:
                c.memset("dve", rawc[rs][:, 0:3], 0.0, [b_rawc[rs]])
            else:
                c.copy("dve", rawc[rs][:, 0:3], halo[:, cc, 0:3], [b_halo], [b_rawc[rs]])
            cw = k.col(l, C_CONVW, 32)
            c.ts("dve", acc[rs], rawc[rs][:, 0:NT], cw[:, cc:cc + 1], None, ALU.mult, None, [b_rawc[rs], k.b_const], [b_acc[rs]])
            for w_ in range(1, 4):
                c.stt(acc[rs], rawc[rs][:, w_:w_ + NT], cw[:, w_ * 8 + cc:w_ * 8 + cc + 1], acc[rs], ALU.mult, ALU.add,
                      [b_rawc[rs], b_acc[rs], k.b_const], [b_acc[rs]])
            if th == 0:
                c.copy("dve", halo[:, cc, 0:3], rawc[rs][:, NT:NT + 3], [b_rawc[rs]], [b_halo])
            ss = stage_slot()
            c.act(stg[ss], acc[rs], AF.Silu, [b_acc[rs], k.b_const], [b_stg[ss]], bias=k.col(l, C_CONVB + cc, 1))
            c.dma("sp", k.mqk_scr[cc, :, t0:t0 + NT], stg[ss], [b_stg[ss]], [k.b_scr], b_sst[ss])
        for cc in (range(8) if "mv" in SEC else []):
            bank0 = proj(OFF_MV + cc * 128, 128)
            ss = stage_slot()
            for hf in range(2):
                c.copy("act", stg[ss][:, hf * 512:(hf + 1) * 512], ps[bank0 + hf], [b_ps[bank0 + hf]], [b_stg[ss]])
            for tl in range(8):
                c.tr(ps6b[:, tl * 128:(tl + 1) * 128], stg[ss][:, tl * 128:(tl + 1) * 128], k.ident_b,
                     [b_stg[ss], k.b_const], [b_ps[6]], inc=(tl == 7))
            vs = cc % 2
            c.copy("dve", vst[vs].rearrange("p a b -> p (a b)"), ps6b, [b_ps[6]], [b_vst[vs]])
            c.dma("sp", k.v_scr[cc // 2, :, th * 8:(th + 1) * 8, (cc % 2) * 128:(cc % 2 + 1) * 128], vst[vs],
                  [b_vst[vs]], [k.b_scr], b_vss[vs])
        for (off, n, scr) in (((OFF_MO, 8, k.sgo_scr), (OFF_GA, 16, k.sga_scr), (OFF_GM, 16, k.sgm_scr)) if "gates" in SEC else []):
            for cc in range(n):
                bank0 = proj(off + cc * 128, 128)
                ss = stage_slot()
                for hf in range(2):
                    c.act(stg[ss][:, hf * 512:(hf + 1) * 512], ps[bank0 + hf], AF.Sigmoid, [b_ps[bank0 + hf]], [b_stg[ss]])
                c.dma("sp", scr[cc, :, t0:t0 + NT], stg[ss], [b_stg[ss]], [k.b_scr], b_sst[ss])
        for (off, cb, dstrow, bb) in (((OFF_MI, C_IB, k.gi, k.b_gi), (OFF_MF, C_FB, k.gf, k.b_gf)) if "gi" in SEC else []):
            bank0 = proj(off, 4)
            for hf in range(2):
                c.act(dstrow[0:4, t0 + hf * 512:t0 + (hf + 1) * 512], ps[bank0 + hf][0:4, :], AF.Identity,
                      [b_ps[bank0 + hf], k.b_const], [bb], bias=k.col(l, cb, 1)[0:4, :])
        c.barrier()
    c.pop_scope()
    ar.reset(m0)


def m2_stage(k, l):
    c, ar = k.c, k.ar
    ps, b_ps = k.ps, k.b_ps
    m0 = ar.mark()
    c.push_scope()
    NS = 4
    qj = [ar.alloc([2, 8, 128], BF16) for _ in range(NS)]
    b_qj = [c.buf("qj%d" % i) for i in range(NS)]
    sc = [ar.alloc([S], F32) for _ in range(NS)]
    b_sc = [c.buf("sc%d" % i) for i in range(NS)]
    NIT = 24
    junk = [ar.alloc([S], BF16) for _ in range(2)]
    b_junk = [c.buf("junk%d" % i) for i in range(2)]
    lo = [ar.alloc([1], F32) for _ in range(2)]
    w0 = [ar.alloc([1], F32) for _ in range(2)]
    midc = [ar.alloc([1], F32) for _ in range(2)]
    cntc = [ar.alloc([1], F32) for _ in range(2)]
    tmpc = [ar.alloc([1], F32) for _ in range(2)]
    Wb = [ar.alloc([NIT + 1], F32) for _ in range(2)]
    nW = [ar.alloc([NIT + 1], F32) for _ in range(2)]
    m8 = [ar.alloc([8], F32) for _ in range(2)]
    pw = ar.alloc([NIT + 1], F32)
    b_bis = [c.buf("bis%d" % i) for i in range(2)]
    b_pw = c.buf("pw")
    for n_ in range(NIT + 1):
        c.memset("dve", pw[:, n_:n_ + 1], 2.0 ** -(n_ + 1), [b_pw])
    thr0 = ar.alloc([1], F32)
    rl = [ar.alloc([512], BF16) for _ in range(4)]
    b_rl = [c.buf("rl%d" % i) for i in range(4)]
    dg = [ar.alloc([8, 128], BF16) for _ in range(NS)]
    b_dg = [c.buf("dg%d" % i) for i in range(NS)]
    negm = [ar.alloc([S], BF16) for _ in range(NS)]
    b_negm = [c.buf("negm%d" % i) for i in range(NS)]
    pT = [ar.alloc([512], BF16) for _ in range(3)]
    b_pT = [c.buf("pT%d" % i) for i in range(3)]
    oraw = [ar.alloc([2, 512], F32) for _ in range(2)]
    b_oraw = [c.buf("oraw%d" % i) for i in range(2)]
    rcp = [ar.alloc([512], F32) for _ in range(2)]
    b_rcp = [c.buf("rcp%d" % i) for i in range(2)]
    ost = [ar.alloc([4, 128], BF16) for _ in range(4)]
    b_ost = [c.buf("ost%d" % i) for i in range(4)]
    b_oss = [c.buf("oss%d" % i) for i in range(4)]
    b_thr0 = c.buf("thr0")
    c.memset("dve", thr0, -1e29, [b_thr0])
    o_v = k.o_scr.rearrange("(h rc) p t -> rc p h t", rc=2)
    id4 = k.ident4_b.rearrange("p a b -> p (a b)")
    cnt = {"idx": 0, "lg": 0, "pt": 0, "os": 0, "hg": 0}

    def scores(j):
        SP = (j + 1) * 128
        qs = j % NS
        c.dma("sp", qj[qs], k.q_scr[j], [k.b_scr], [b_qj[qs]], b_qj[qs])
        scj = sc[qs]
        b_scj = b_sc[qs]
        for h in range(HI):
            c.ts("pool", dg[qs][:, h, :], k.ident_f, k.iw[:, j, h:h + 1], None, ALU.mult, None, [k.b_const, k.b_iw], [b_dg[qs]])
        nblk = (SP + 511) // 512
        for blk in range(nblk):
            w_ = min(512, SP - blk * 512)
            for h in range(HI):
                ch, pb = h // 2, (h % 2) * 64
                ib = cnt["idx"] % 2
                rs = cnt["idx"] % 4
                cnt["idx"] += 1
                c.mm(ps[ib][:, 0:w_], k.iqT[pb:pb + 64, ch, j * 128:(j + 1) * 128], k.ikT[pb:pb + 64, blk * 512:blk * 512 + w_],
                     True, True, [k.b_iqT, k.b_ikT], [b_ps[ib]])
                c.act(rl[rs][:, 0:w_], ps[ib][:, 0:w_], AF.Relu, [b_ps[ib]], [b_rl[rs]])
                c.mm(ps[7][:, 0:w_], dg[qs][:, h, :], rl[rs][:, 0:w_], h == 0, h == HI - 1, [b_dg[qs], b_rl[rs]], [b_ps[7]])
            c.copy("act", scj[:, blk * 512:blk * 512 + w_], ps[7][:, 0:w_], [b_ps[7]], [b_scj])
        c.tt("dve", scj[:, j * 128:(j + 1) * 128], scj[:, j * 128:(j + 1) * 128], k.cneg_f, ALU.add, [b_scj, k.b_const], [b_scj])

    def bisect(js):
        js = [j for j in js if j >= 2]
        for j in js:
            x = j % 2
            qs = j % NS
            SP = (j + 1) * 128
            sct = sc[qs][:, 0:SP]
            c.op("dve", (lambda o, i_: (lambda e: e.max(out=o, in_=i_)))(m8[x], sct), reads=[b_sc[qs]], writes=[b_bis[x]])
            c.op("dve", (lambda o, i_: (lambda e: e.tensor_reduce(out=o, in_=i_, axis=AX.X, op=ALU.min)))(lo[x], sc[qs][:, 0:j * 128]),
                 reads=[b_sc[qs]], writes=[b_bis[x]])
            c.tt("dve", w0[x], m8[x][:, 0:1], lo[x], ALU.subtract, [b_bis[x]], [b_bis[x]])
            c.ts("dve", Wb[x], pw, w0[x], None, ALU.mult, None, [b_bis[x], b_pw], [b_bis[x]])
            c.ts("dve", nW[x], Wb[x], -1.0, None, ALU.mult, None, [b_bis[x]], [b_bis[x]])
            c.tt("dve", midc[x], lo[x], Wb[x][:, 0:1], ALU.add, [b_bis[x]], [b_bis[x]])
        for n_ in range(NIT):
            for j in js:
                x = j % 2
                qs = j % NS
                SP = (j + 1) * 128
                c.op("dve", (lambda o, i_, m_, a_: (lambda e: e.tensor_scalar(out=o, in0=i_, scalar1=m_, scalar2=0.0, op0=ALU.is_ge,
                                                                             op1=ALU.add, accum_out=a_)))(
                    junk[x][:, 0:SP], sc[qs][:, 0:SP], midc[x], cntc[x]), reads=[b_sc[qs], b_bis[x]], writes=[b_bis[x], b_junk[x]])
            for j in js:
                x = j % 2
                c.ts("dve", tmpc[x], cntc[x], float(TOPK) - 0.5, Wb[x][:, n_:n_ + 1], ALU.is_ge, ALU.mult, [b_bis[x]], [b_bis[x]])
                c.stt(midc[x], tmpc[x], nW[x][:, n_ + 1:n_ + 2], midc[x], ALU.add, ALU.add, [b_bis[x]], [b_bis[x]])
        for j in js:
            x = j % 2
            c.tt("dve", lo[x], midc[x], nW[x][:, NIT:NIT + 1], ALU.add, [b_bis[x]], [b_bis[x]])

    def negmask(j):
        SP = (j + 1) * 128
        qs = j % NS
        if j >= 2:
            thr, b_thr = lo[j % 2], b_bis[j % 2]
        else:
            thr, b_thr = thr0, b_thr0
        c.ts("dve", negm[qs][:, 0:SP], sc[qs][:, 0:SP], thr, NEG, ALU.is_lt, ALU.mult, [b_sc[qs], b_thr], [b_negm[qs]])

    def prep_pair(m):
        js = (2 * m, 2 * m + 1)
        for j in js:
            scores(j)
        bisect(js)
        for j in js:
            negmask(j)

    def attn(j):
        qs = j % NS
        nm = negm[qs]
        b_nm = b_negm[qs]
        for hg in range(2):
            lbs = {}

            def logits(i):
                lb = 2 + cnt["lg"] % 2
                cnt["lg"] += 1
                lbs[i] = lb
                for rc in range(2):
                    c.mm(ps[lb], k.ckvT[:, rc, i * 128:(i + 1) * 128], qj[qs][:, rc, hg * 4:(hg + 1) * 4, :].rearrange("p a b -> p (a b)"),
                         rc == 0, False, [k.b_ckvT, b_qj[qs]], [b_ps[lb]], inc=False)
                near = (j - i <= 1)
                if near:
                    for tbx in (k.TBh, k.TBl):
                        c.mm(ps[lb], k.ident_b, tbx[:, j - i, hg * 4:(hg + 1) * 4, :].rearrange("p a b -> p (a b)"), False, False,
                             [k.b_TB, k.b_const], [b_ps[lb]], inc=False)
                c.mm(ps[lb], nm[:, i * 128:(i + 1) * 128], id4, False, True, [b_nm, k.b_const], [b_ps[lb]])

            logits(0)
            for i in range(j + 1):
                if i + 1 <= j:
                    logits(i + 1)
                lb = lbs[i]
                pt = cnt["pt"] % 3
                cnt["pt"] += 1
                c.act(pT[pt], ps[lb], AF.Exp, [b_ps[lb]], [b_pT[pt]])
                for rc in range(2):
                    c.mm(ps[4 + rc], k.ckv[:, i, rc * 128:(rc + 1) * 128], pT[pt], i == 0, i == j, [k.b_ckv, b_pT[pt]], [b_ps[4 + rc]],
                         inc=False)
                c.mm(ps[6], k.ones_b, pT[pt], i == 0, i == j, [k.b_const, b_pT[pt]], [b_ps[6]])
            hs = cnt["hg"] % 2
            cnt["hg"] += 1
            for rc in range(2):
                c.copy("act", oraw[hs][:, rc, :], ps[4 + rc], [b_ps[4 + rc]], [b_oraw[hs]])
            c.act(rcp[hs], ps[6], AF.Ln, [b_ps[6]], [b_rcp[hs]])
            c.act(rcp[hs], rcp[hs], AF.Exp, [b_rcp[hs]], [b_rcp[hs]], scale=-1.0)
            for rc in range(2):
                os_ = cnt["os"] % 4
                cnt["os"] += 1
                c.tt("pool", ost[os_].rearrange("p a b -> p (a b)"), oraw[hs][:, rc, :], rcp[hs], ALU.mult, [b_oraw[hs], b_rcp[hs]],
                     [b_ost[os_]])
                c.dma("sp", o_v[rc, :, hg * 4:(hg + 1) * 4, j * 128:(j + 1) * 128], ost[os_], [b_ost[os_]], [k.b_scr], b_oss[os_])

    prep_pair(0)
    for m in range(8):
        if m + 1 < 8:
            prep_pair(m + 1)
        attn(2 * m)
        attn(2 * m + 1)
    c.barrier()
    c.pop_scope()
    ar.reset(m0)


def m3_stage(k, l):
    c, ar = k.c, k.ar
    ps, b_ps = k.ps, k.b_ps
    m0 = ar.mark()
    c.push_scope()
    e1 = ar.alloc([S], F32)
    l1 = ar.alloc([S], F32)
    Lc = ar.alloc([S], F32)
    a_ = e1
    M_ = l1
    nM = ar.alloc([S], F32)
    cl = ar.alloc([S], F32)
    zer = cl
    b_rows = c.buf("rows")
    Rh = ar.alloc([S], F32)
    Qh = ar.alloc([S], F32)
    Ch = ar.alloc([S], F32)
    b_Rh, b_Qh, b_Ch = c.buf("Rh"), c.buf("Qh"), c.buf("Ch")
    trineg = ar.alloc([128], F32)
    b_trineg = c.buf("trineg")
    acol = ar.alloc([16], F32)
    b_acol = c.buf("acol")
    nmbc = [ar.alloc([512], F32) for _ in range(2)]
    b_nmbc = [c.buf("nmbc%d" % i) for i in range(2)]
    qh = ar.alloc([S], BF16)
    kh = ar.alloc([S], BF16)
    vh = ar.alloc([16, 256], BF16)
    b_qh, b_kh, b_vh = c.buf("qh"), c.buf("kh"), c.buf("vh")
    clbc = ar.alloc([S], F32)
    b_clbc = c.buf("clbc")
    numT = ar.alloc([2, S], F32)
    b_numT = c.buf("numT")
    den = ar.alloc([S], F32)
    b_den = c.buf("den")
    ex = [ar.alloc([512], F32) for _ in range(2)]
    b_ex = [c.buf("ex%d" % i) for i in range(2)]
    dtl = [ar.alloc([512], F32) for _ in range(2)]
    b_dtl = [c.buf("dtl%d" % i) for i in range(2)]
    ptl = [ar.alloc([512], BF16) for _ in range(3)]
    b_ptl = [c.buf("ptl%d" % i) for i in range(3)]
    maskv = [ar.alloc([512], F32) for _ in range(4)]
    sq2 = ar.alloc([S], F32)
    b_sq2 = c.buf("sq2")
    rn, b_rn = clbc, b_clbc
    sgo = [ar.alloc([S], BF16) for _ in range(2)]
    b_sgo = [c.buf("sgo%d" % i) for i in range(2)]
    hst = [ar.alloc([S], BF16) for _ in range(2)]
    b_hst = [c.buf("hmst%d" % i) for i in range(2)]
    b_hss = [c.buf("hmss%d" % i) for i in range(2)]
    R4 = slice(0, 4)
    c.act(e1[R4, :], k.gf[R4, :], AF.Exp, [k.b_gf], [b_rows], scale=-1.0)
    c.act(l1[R4, :], e1[R4, :], AF.Ln, [b_rows], [b_rows], bias=k.one_col[R4, :])
    c.memset("dve", zer[0:4, :], 0.0, [b_rows])
    c.op("dve", (lambda o, d0, d1: (lambda e: e.tensor_tensor_scan(out=o, data0=d0, data1=d1, initial=0.0, op0=ALU.add, op1=ALU.add)))(
        Lc[R4, :], l1[R4, :], zer[R4, :]), reads=[b_rows], writes=[b_rows])
    c.tt("dve", a_[R4, :], k.gi[R4, :], Lc[R4, :], ALU.add, [k.b_gi, b_rows], [b_rows])
    c.op("dve", (lambda o, d0, d1: (lambda e: e.tensor_tensor_scan(out=o, data0=d0, data1=d1, initial=0.0, op0=ALU.max, op1=ALU.max)))(
        M_[R4, :], a_[R4, :], a_[R4, :]), reads=[b_rows], writes=[b_rows])
    c.ts("dve", nM[R4, :], M_[R4, :], -1.0, None, ALU.mult, None, [b_rows], [b_rows])
    c.tt("dve", cl[R4, :], Lc[R4, :], M_[R4, :], ALU.subtract, [b_rows], [b_rows])
    c.act(cl[R4, :], cl[R4, :], AF.Exp, [b_rows], [b_rows])
    c.ts("dve", trineg, k.cst[:, 256:384], -1.0, -NEG, ALU.add, ALU.mult, [k.b_const], [b_trineg])
    for d_ in range(4):
        c.memset("dve", maskv[d_], 0.0, [b_trineg])
        if d_ > 0:
            c.memset("dve", maskv[d_][:, 0:d_ * 128], NEG, [b_trineg])
        c.copy("dve", maskv[d_][:, d_ * 128:(d_ + 1) * 128], trineg, [b_trineg], [b_trineg])
    hm_v = k.hm_scr
    for h in range(MH):
        c.dma("sp", Rh[0:1, :], a_[h:h + 1, :], [b_rows], [b_Rh], b_Rh)
        c.dma("sp", Qh[0:1, :], nM[h:h + 1, :], [b_rows], [b_Qh], b_Qh)
        for i in range(16):
            c.tr(ps[7][:, i:i + 1], Rh[0:1, i * 128:(i + 1) * 128], k.ident_f[0:1, 0:1], [b_Rh, k.b_const], [b_ps[7]], inc=(i == 15))
        c.copy("dve", acol, ps[7][:, 0:16], [b_ps[7]], [b_acol])
        c.dma("sp", Ch[0:1, :], cl[h:h + 1, :], [b_rows], [b_Ch], b_Ch)
        c.dma("sp", qh, k.mqk_scr[h], [k.b_scr], [b_qh], b_qh)
        c.dma("sp", kh, k.mqk_scr[4 + h], [k.b_scr], [b_kh], b_kh)
        c.dma("sp", vh, k.v_scr[h], [k.b_scr], [b_vh], b_vh)
        for blk in range(4):
            c.mm(ps[7], k.ones_f[0:1, :], Ch[0:1, blk * 512:(blk + 1) * 512], True, True, [b_Ch, k.b_const], [b_ps[7]])
            c.copy("act", clbc[:, blk * 512:(blk + 1) * 512], ps[7], [b_ps[7]], [b_clbc])
        cnt = 0
        for tb in range(4):
            ni = 4 * tb + 4
            ts_ = slice(tb * 512, (tb + 1) * 512)
            slots = {}
            c.mm(ps[2], k.ones_f[0:1, :], Qh[0:1, ts_], True, True, [b_Qh, k.b_const], [b_ps[2]])
            c.copy("act", nmbc[tb % 2], ps[2], [b_ps[2]], [b_nmbc[tb % 2]])

            def front(i):
                nonlocal cnt
                sb = cnt % 2
                pt = cnt % 3
                cnt += 1
                slots[i] = pt
                c.mm(ps[sb], kh[:, i * 128:(i + 1) * 128], qh[:, ts_], True, True, [b_kh, b_qh], [b_ps[sb]])
                if i >= 4 * tb:
                    c.tt("dve", ex[sb], nmbc[tb % 2], maskv[i - 4 * tb], ALU.add, [b_nmbc[tb % 2], b_trineg], [b_ex[sb]])
                    c.act(dtl[sb], ex[sb], AF.Exp, [b_ex[sb], b_acol], [b_dtl[sb]], bias=acol[:, i:i + 1])
                else:
                    c.act(dtl[sb], nmbc[tb % 2], AF.Exp, [b_nmbc[tb % 2], b_acol], [b_dtl[sb]], bias=acol[:, i:i + 1])
                c.stt(ptl[pt], ps[sb], float(DK ** -0.5), dtl[sb], ALU.mult, ALU.mult, [b_ps[sb], b_dtl[sb]], [b_ptl[pt]])

            front(0)
            for i in range(ni):
                if i + 1 < ni:
                    front(i + 1)
                pt = slots[i]
                for cc in range(2):
                    c.mm(ps[4 + cc], vh[:, i, cc * 128:(cc + 1) * 128], ptl[pt], i == 0, i == ni - 1, [b_vh, b_ptl[pt]],
                         [b_ps[4 + cc]], inc=False)
                c.mm(ps[6], k.ones_b, ptl[pt], i == 0, i == ni - 1, [k.b_const, b_ptl[pt]], [b_ps[6]])
            for cc in range(2):
                c.copy("act", numT[:, cc, ts_], ps[4 + cc], [b_ps[4 + cc]], [b_numT])
            c.copy("act", den[:, ts_], ps[6], [b_ps[6]], [b_den])
        c.ts("dve", sq2, den, -1.0, None, ALU.mult, None, [b_den], [b_sq2])
        c.tt("dve", den, den, sq2, ALU.max, [b_den, b_sq2], [b_den])
        c.tt("dve", den, den, clbc, ALU.max, [b_den, b_clbc], [b_den])
        c.act(den, den, AF.Ln, [b_den], [b_den])
        c.act(den, den, AF.Exp, [b_den], [b_den], scale=-1.0)
        for cc in range(2):
            c.tt("dve", numT[:, cc, :], numT[:, cc, :], den, ALU.mult, [b_numT, b_den], [b_numT])
        for cc in range(2):
            c.act(sq2, numT[:, cc, :], AF.Square, [b_numT], [b_sq2])
            for blk in range(4):
                c.mm(ps[blk], k.ones_f, sq2[:, blk * 512:(blk + 1) * 512], cc == 0, cc == 1, [b_sq2, k.b_const], [b_ps[blk]])
        for blk in range(4):
            c.act(rn[:, blk * 512:(blk + 1) * 512], ps[blk], AF.Ln, [b_ps[blk]], [b_rn], bias=k.eps_col, scale=1.0 / DV)
        c.act(rn, rn, AF.Exp, [b_rn], [b_rn], scale=-0.5)
        for cc in range(2):
            ch = h * 2 + cc
            s_ = ch % 2
            c.dma("sp", sgo[s_], k.sgo_scr[ch], [k.b_scr], [b_sgo[s_]], b_sgo[s_])
            c.stt(numT[:, cc, :], numT[:, cc, :], k.col(l, C_MON + ch, 1), rn, ALU.mult, ALU.mult, [b_numT, b_rn, k.b_const], [b_numT])
            c.tt("dve", hst[s_], numT[:, cc, :], sgo[s_], ALU.mult, [b_numT, b_sgo[s_]], [b_hst[s_]])
            c.dma("sp", hm_v[ch], hst[s_], [b_hst[s_]], [k.b_scr], b_hss[s_])
    c.barrier()
    c.pop_scope()
    ar.reset(m0)


def m4_stage(k, l):
    c, ar = k.c, k.ar
    ps, b_ps = k.ps, k.b_ps
    NT = 1024
    m0 = ar.mark()
    c.push_scope()
    xatt = ar.alloc([16, NT], BF16)
    xmem = ar.alloc([8, NT], BF16)
    mrg = ar.alloc([16, NT], BF16)
    b_xatt, b_xmem = c.buf("xatt"), c.buf("xmem")
    b_mrg = [c.buf("mrg%d" % i) for i in range(16)]
    wa = [ar.alloc([16, 128], BF16) for _ in range(2)]
    wm = [ar.alloc([8, 128], BF16) for _ in range(2)]
    wo = [ar.alloc([16, 128], BF16) for _ in range(2)]
    b_wa = [c.buf("wa%d" % i) for i in range(2)]
    b_wm = [c.buf("wm%d" % i) for i in range(2)]
    b_wo = [c.buf("wo%d" % i) for i in range(2)]
    sga = [ar.alloc([NT], BF16) for _ in range(2)]
    sgm = [ar.alloc([NT], BF16) for _ in range(2)]
    b_sga = [c.buf("sga%d" % i) for i in range(2)]
    b_sgm = [c.buf("sgm%d" % i) for i in range(2)]
    t1 = [ar.alloc([NT], F32) for _ in range(2)]
    b_t1 = [c.buf("t1%d" % i) for i in range(2)]
    t2 = [ar.alloc([NT], F32) for _ in range(2)]
    b_t2 = [c.buf("t2%d" % i) for i in range(2)]
    hres = [ar.alloc([NT], F32) for _ in range(2)]
    b_hres = [c.buf("hres%d" % i) for i in range(2)]
    b_hst = [c.buf("hst%d" % i) for i in range(2)]
    wav = k.w["w_att_out"][l].rearrange("(kc p) m -> p kc m", p=128)
    wmv = k.w["w_mem_out"][l].rearrange("(kc p) m -> p kc m", p=128)
    wov = k.w["w_out"][l].rearrange("(kc p) m -> p kc m", p=128)
    o_in = k.o_scr.rearrange("c p t -> p c t")
    hm_in = k.hm_scr.rearrange("c p t -> p c t")
    for th in range(2):
        t0 = th * NT
        for g in range(4):
            c.dma("sp", xatt[:, g * 4:(g + 1) * 4, :], o_in[:, g * 4:(g + 1) * 4, t0:t0 + NT], [k.b_scr], [b_xatt], b_xatt)
        for g in range(2):
            c.dma("sp", xmem[:, g * 4:(g + 1) * 4, :], hm_in[:, g * 4:(g + 1) * 4, t0:t0 + NT], [k.b_scr], [b_xmem], b_xmem)
        for dc in range(16):
            sl = dc % 2
            c.dma("pool", wa[sl], wav[:, :, dc * 128:(dc + 1) * 128], [], [b_wa[sl]], b_wa[sl])
            c.dma("pool", wm[sl], wmv[:, :, dc * 128:(dc + 1) * 128], [], [b_wm[sl]], b_wm[sl])
            c.dma("sp", sga[sl], k.sga_scr[dc, :, t0:t0 + NT], [k.b_scr], [b_sga[sl]], b_sga[sl])
            c.dma("sp", sgm[sl], k.sgm_scr[dc, :, t0:t0 + NT], [k.b_scr], [b_sgm[sl]], b_sgm[sl])
            pset = sl * 4
            for hf in range(2):
                for kc in range(16):
                    c.mm(ps[pset + hf], wa[sl][:, kc, :], xatt[:, kc, hf * 512:(hf + 1) * 512], kc == 0, kc == 15,
                         [b_wa[sl], b_xatt], [b_ps[pset + hf]], inc=(kc == 15))
            for hf in range(2):
                for kc in range(8):
                    c.mm(ps[pset + 2 + hf], wm[sl][:, kc, :], xmem[:, kc, hf * 512:(hf + 1) * 512], kc == 0, kc == 7,
                         [b_wm[sl], b_xmem], [b_ps[pset + 2 + hf]], inc=(kc == 7))
            for hf in range(2):
                hs = slice(hf * 512, (hf + 1) * 512)
                c.tt("dve", t1[sl][:, hs], ps[pset + hf], sga[sl][:, hs], ALU.mult, [b_ps[pset + hf], b_sga[sl]], [b_t1[sl]])
                c.tt("dve", t2[sl][:, hs], ps[pset + 2 + hf], sgm[sl][:, hs], ALU.mult, [b_ps[pset + 2 + hf], b_sgm[sl]], [b_t2[sl]])
            c.tt("pool", mrg[:, dc, :], t1[sl], t2[sl], ALU.add, [b_t1[sl], b_t2[sl]], [b_mrg[dc]])
        for dc in range(16):
            sl = dc % 2
            c.dma("pool", wo[sl], wov[:, :, dc * 128:(dc + 1) * 128], [], [b_wo[sl]], b_wo[sl])
            c.dma("sp", hres[sl], k.hbuf[dc * 128:(dc + 1) * 128, t0:t0 + NT], [k.b_h], [b_hres[sl]], b_hres[sl])
            pset = sl * 2
            for hf in range(2):
                for kc in range(16):
                    c.mm(ps[pset + hf], wo[sl][:, kc, :], mrg[:, kc, hf * 512:(hf + 1) * 512], kc == 0, kc == 15,
                         [b_wo[sl], b_mrg[kc]], [b_ps[pset + hf]], inc=(kc == 15))
            for hf in range(2):
                hs = slice(hf * 512, (hf + 1) * 512)
                c.tt("dve", hres[sl][:, hs], ps[pset + hf], hres[sl][:, hs], ALU.add, [b_ps[pset + hf], b_hres[sl]], [b_hres[sl]])
            c.dma("sp", k.hbuf[dc * 128:(dc + 1) * 128, t0:t0 + NT], hres[sl], [b_hres[sl]], [k.b_h], b_hst[sl])
        c.barrier()
    c.pop_scope()
    ar.reset(m0)


def full_plan(n_layers=NL):
    plan = []
    for l in range(n_layers):
        plan.append(("ffn", l, 1, "x" if l == 0 else "h", "h"))
        plan += [("m1", l), ("m2", l), ("m3", l), ("m4", l)]
        plan.append(("ffn", l, 2, "h", "o" if l == n_layers - 1 else "h"))
    return plan


_WNAMES = ["ffn1_w_gu", "ffn1_w_down", "w_in", "w_att_out", "w_mem_out", "w_out", "ffn2_w_gu", "ffn2_w_down"]
_NC_CACHE = {}


def make_maps(inp, batch_ids):
    cst, tb, rb31 = host_consts(np.asarray(inp["rel_bias"], np.float32))
    cols = host_cols(inp)
    shared = {"cols": cols, "cst": cst, "tbraw": tb, "rb31": rb31}
    for n in _WNAMES:
        a = np.asarray(inp[n], np.float32)
        shared[n] = np.ascontiguousarray(a.reshape(NL, -1, a.shape[-1]))
    maps = []
    for b in batch_ids:
        m = dict(shared)
        m["xT"] = np.ascontiguousarray(np.asarray(inp["x"][b], np.float32).T)
        maps.append(m)
    return maps


def kernel(**inputs):
    if "nc" not in _NC_CACHE:
        _NC_CACHE["nc"] = build(full_plan(NL))
    nc = _NC_CACHE["nc"]
    B = inputs["x"].shape[0]
    maps = make_maps(inputs, list(range(B)))
    res = run_bass_kernel_spmd(nc, maps, core_ids=list(range(B)))
    out = np.stack([np.asarray(r["outT"]).T for r in res.results], axis=0)
    return np.ascontiguousarray(out.astype(np.float32))
```

```python
import math
from contextlib import ExitStack
import numpy as np
import concourse.bass as bass
import concourse.mybir as mybir
from concourse.bass_utils import run_bass_kernel_spmd

F32 = mybir.dt.float32
BF16 = mybir.dt.bfloat16
AF = mybir.ActivationFunctionType
ALU = mybir.AluOpType
AX = mybir.AxisListType

D = 2048
S = 2048
NL = 4
FF = 5504
NFC = FF // 128
H = 8
R = 256
HI = 8
DI = 64
TOPK = 256
MH = 4
DK = 128
DV = 256
EPS = 1e-6
D_IN = 10064
OFF_AQ, OFF_CKV, OFF_IQ, OFF_IK, OFF_IW, OFF_MQK, OFF_MV, OFF_MO, OFF_MI, OFF_MF, OFF_GA, OFF_GM = (
    0, 2048, 2304, 2816, 2880, 2888, 3912, 4936, 5960, 5964, 5968, 8016)
NEG = -30000.0

C_FFN1, C_MIX, C_FFN2 = 0, 16, 32
C_QN, C_KVN = 48, 50
C_CONVW = 52
C_CONVB = 84
C_MON = 92
C_IB, C_FB = 100, 101
NCOLS = 104


class Buf:
    __slots__ = ("name", "w", "r", "multi", "dsem", "dcnt")

    def __init__(self, name, multi=False):
        self.name = name
        self.w = {}
        self.r = {}
        self.multi = multi
        self.dsem = None
        self.dcnt = 0


class Queue:
    def __init__(self, name, sem):
        self.name = name
        self.sem = sem
        self.cnt = 0
        self.known = {}
        self.ops = []


class Ctx:
    def __init__(self, nc, es):
        self.nc = nc
        self.es = es
        self.q = {}
        for n in ("pe", "act", "dve", "pool", "sp"):
            self.q[n] = Queue(n, es.enter_context(nc.semaphore("q_" + n)))
        self.bufs = []
        self.dma_sems = []
        self.sem_pool = []
        self.scopes = []
        self.ninstr = 0
        self.nsem = 0

    def buf(self, name, multi=False):
        b = Buf(name, multi)
        self.bufs.append(b)
        if self.scopes:
            self.scopes[-1].append(b)
        return b

    def push_scope(self):
        self.scopes.append([])

    def pop_scope(self):
        for b in self.scopes.pop():
            self.bufs.remove(b)
            if b.dsem is not None:
                self.dma_sems.remove(b)
                self.sem_pool.append((b.dsem, b.dcnt))

    def _dsem(self, b):
        if b.dsem is None:
            if self.sem_pool:
                b.dsem, b.dcnt = self.sem_pool.pop()
            else:
                self.nsem += 1
                b.dsem = self.es.enter_context(self.nc.semaphore("d%d" % self.nsem))
            self.dma_sems.append(b)
        return b.dsem

    def op(self, qn, fn, reads=(), writes=(), dma=None, inc=True):
        q = self.q[qn]
        deps = {}
        own = self._dsem(dma) if dma is not None else None

        def merge(d, same_ok):
            for s, v in d.items():
                if s is own:
                    continue
                if (s is q.sem) and dma is None:
                    if qn == "pe" or not same_ok:
                        continue
                if deps.get(s, 0) < v:
                    deps[s] = v
        for b in reads:
            merge(b.w, True)
        for b in writes:
            if not b.multi:
                merge(b.w, False)
            merge(b.r, False)
        waits = []
        for s, v in deps.items():
            if q.known.get(s, 0) >= v:
                continue
            q.known[s] = v
            waits.append((s, v))
        if dma is not None:
            sem = self._dsem(dma)
            dma.dcnt += 16
            val = dma.dcnt
            assert val < 65000, ("dma sem overflow", dma.name)
            q.ops.append((waits, fn, sem, 16))
        else:
            sem = q.sem
            if inc:
                q.cnt += 1
                val = q.cnt
                assert val < 65000, ("sem overflow", qn)
                q.ops.append((waits, fn, sem, 1))
            else:
                val = q.cnt + 1
                q.ops.append((waits, fn, None, 0))
        self.ninstr += 1 + len(waits)
        for b in reads:
            if b.r.get(sem, 0) < val:
                b.r[sem] = val
        for b in writes:
            if b.multi:
                if b.w.get(sem, 0) < val:
                    b.w[sem] = val
            else:
                b.w = {sem: val}
                b.r = {}

    def dma(self, qn, out, in_, reads, writes, dma):
        self.op(qn, lambda e: e.dma_start(out=out, in_=in_), reads=reads, writes=writes, dma=dma)

    def act(self, out, in_, func, reads, writes, bias=None, scale=1.0, accum_out=None):
        kw = {}
        if bias is not None:
            kw["bias"] = bias
        if accum_out is not None:
            kw["accum_out"] = accum_out
        self.op("act", lambda e: e.activation(out=out, in_=in_, func=func, scale=scale, **kw), reads=reads, writes=writes)

    def mm(self, out, lhsT, rhs, start, stop, reads, writes, inc=True):
        self.op("pe", lambda e: e.matmul(out, lhsT=lhsT, rhs=rhs, start=start, stop=stop), reads=reads, writes=writes, inc=inc)

    def tr(self, out, in_, ident, reads, writes, inc=True):
        self.op("pe", lambda e: e.transpose(out, in_, ident), reads=reads, writes=writes, inc=inc)

    def tt(self, qn, out, in0, in1, op, reads, writes):
        self.op(qn, lambda e: e.tensor_tensor(out=out, in0=in0, in1=in1, op=op), reads=reads, writes=writes)

    def stt(self, out, in0, scalar, in1, op0, op1, reads, writes):
        self.op("dve", lambda e: e.scalar_tensor_tensor(out=out, in0=in0, scalar=scalar, in1=in1, op0=op0, op1=op1),
                reads=reads, writes=writes)

    def ts(self, qn, out, in0, s1, s2, op0, op1, reads, writes):
        if s2 is None:
            self.op(qn, lambda e: e.tensor_scalar(out=out, in0=in0, scalar1=s1, scalar2=None, op0=op0), reads=reads, writes=writes)
        else:
            self.op(qn, lambda e: e.tensor_scalar(out=out, in0=in0, scalar1=s1, scalar2=s2, op0=op0, op1=op1),
                    reads=reads, writes=writes)

    def copy(self, qn, out, in_, reads, writes):
        if qn == "act":
            self.op(qn, lambda e: e.activation(out=out, in_=in_, func=AF.Copy), reads=reads, writes=writes)
        else:
            self.op(qn, lambda e: e.tensor_copy(out=out, in_=in_), reads=reads, writes=writes)

    def recip(self, out, in_, reads, writes):
        self.op("dve", lambda e: e.reciprocal(out=out, in_=in_), reads=reads, writes=writes)

    def memset(self, qn, out, val, writes):
        self.op(qn, lambda e: e.memset(out, val), reads=[], writes=writes)

    def barrier(self):
        toks = {}
        for qn, q in self.q.items():
            if q.cnt > 0:
                toks[q.sem] = q.cnt
        for b in self.dma_sems:
            if b.dcnt > 0:
                toks[b.dsem] = b.dcnt
        for qn, q in self.q.items():
            waits = []
            for s, v in toks.items():
                if s is q.sem and qn == "pe":
                    continue
                if q.known.get(s, 0) >= v:
                    continue
                q.known[s] = v
                waits.append((s, v))
            if waits:
                q.ops.append((waits, None, None, 0))
                self.ninstr += len(waits)
        for b in self.bufs:
            b.w = {}
            b.r = {}

    def emit(self):
        nc = self.nc
        with nc.Block() as block:
            def run(q):
                def f(e):
                    for waits, fn, sem, inc in q.ops:
                        for s, v in waits:
                            e.wait_ge(s, v)
                        if fn is not None:
                            ins = fn(e)
                            if sem is not None:
                                ins.then_inc(sem, inc)
                return f
            block.tensor(run(self.q["pe"]))
            block.scalar(run(self.q["act"]))
            block.vector(run(self.q["dve"]))
            block.gpsimd(run(self.q["pool"]))
            block.sync(run(self.q["sp"]))


class Arena:
    def __init__(self, nc, es, nbytes):
        self.t = es.enter_context(nc.sbuf_tensor("arena", [128, nbytes // 4], F32))
        self.nbytes = nbytes
        self.top = 0

    def mark(self):
        return self.top

    def reset(self, m):
        self.top = m

    def alloc(self, shape, dtype):
        esz = 4 if dtype == F32 else 2
        n = 1
        for s in shape:
            n *= s
        nb = (n * esz + 31) // 32 * 32
        assert self.top + nb <= self.nbytes, ("arena overflow", self.top, nb, self.nbytes)
        o = self.top // 4
        ap = self.t[:, o:o + nb // 4]
        self.top += nb
        if dtype != F32:
            ap = ap.bitcast(dtype)
        ap = ap[:, 0:n]
        if len(shape) == 2:
            ap = ap.rearrange("p (a b) -> p a b", a=shape[0])
        elif len(shape) == 3:
            ap = ap.rearrange("p (a b c) -> p a b c", a=shape[0], b=shape[1])
        return ap


class K:
    dbg_on = False

    def dump(self, name, ap, bufs, dtype=F32):
        if not self.dbg_on or True:
            return
        shp = list(ap.shape)
        d = self.nc.dram_tensor("dbg_" + name, shp, dtype, kind="ExternalOutput").ap()
        b = self.c.buf("dbg_" + name)
        self.c.dma("sp", d, ap, bufs, [], b)


def ffn_stage(k, src, dst, gcol, wgu, wd):
    c, ar, nc = k.c, k.ar, k.nc
    NT = 1024
    m0 = ar.mark()
    c.push_scope()
    actT = ar.alloc([NFC, NT], BF16)
    hk = ar.t[:, m0 // 4:m0 // 4 + 16 * NT].rearrange("p (a b) -> p a b", a=16)
    b_hk = [c.buf("hk%d" % i) for i in range(4)]
    wgt = [ar.alloc([16, 128], BF16) for _ in range(3)]
    wut = [ar.alloc([16, 128], BF16) for _ in range(3)]
    regU = ar.mark()
    uT = ar.alloc([16, NT], BF16)
    regX = ar.mark()
    hld = [ar.alloc([NT], F32) for _ in range(2)]
    sqt = [ar.alloc([NT], F32) for _ in range(2)]
    rstd = ar.alloc([NT], F32)
    tmp = ar.alloc([NT], F32)
    ar.reset(regX)
    sgt = [ar.alloc([NT], F32) for _ in range(2)]
    ar.reset(regU)
    wdt = [ar.alloc([NFC, 128], BF16) for _ in range(2)]
    hres = [ar.alloc([NT], F32) for _ in range(2)]
    ar.top = max(ar.top, regX + 6 * NT * 4)

    b_act = [c.buf("act%d" % i) for i in range(NFC)]
    b_wg = [c.buf("wg%d" % i) for i in range(3)]
    b_wu = [c.buf("wu%d" % i) for i in range(3)]
    b_uT = [c.buf("uT%d" % i) for i in range(16)]
    b_hld = [c.buf("hld%d" % i) for i in range(2)]
    b_sqt = [c.buf("sqt%d" % i) for i in range(2)]
    b_rstd = c.buf("rstd")
    b_tmp = c.buf("tmp")
    b_sgt = [c.buf("sgt%d" % i) for i in range(2)]
    b_wd = [c.buf("wd%d" % i) for i in range(2)]
    b_hres = [c.buf("hres%d" % i) for i in range(2)]
    b_hst = [c.buf("hst%d" % i) for i in range(2)]
    ps, b_ps = k.ps, k.b_ps
    wgu_v = wgu.rearrange("(kc p) m -> p kc m", p=128)
    wd_v = wd.rearrange("(fc p) m -> p fc m", p=128)

    for tb in range(S // NT):
        t0 = tb * NT
        for f in range(3):
            c.dma("pool", wgt[f], wgu_v[:, :, f * 128:(f + 1) * 128], [], [b_wg[f]], b_wg[f])
            c.dma("pool", wut[f], wgu_v[:, :, FF + f * 128:FF + (f + 1) * 128], [], [b_wu[f]], b_wu[f])
        for cc in range(16):
            c.dma("sp", hk[:, cc, :], src[cc * 128:(cc + 1) * 128, t0:t0 + NT], [k.b_h], [b_hk[cc // 4]], b_hk[cc // 4])
        for cc in range(16):
            sl = cc % 2
            c.act(sqt[sl], hk[:, cc, :], AF.Square, [b_hk[cc // 4]], [b_sqt[sl]])
            for hf in range(2):
                c.mm(ps[hf], k.ones_f, sqt[sl][:, hf * 512:(hf + 1) * 512], cc == 0, cc == 15,
                     [b_sqt[sl]], [b_ps[hf]], inc=(hf == 1))
        for hf in range(2):
            c.act(tmp[:, hf * 512:(hf + 1) * 512], ps[hf], AF.Ln, [b_ps[hf]], [b_tmp], bias=k.eps_col, scale=1.0 / D)
        c.act(rstd, tmp, AF.Exp, [b_tmp], [b_rstd], scale=-0.5)
        for cc in range(16):
            c.stt(uT[:, cc, :], hk[:, cc, :], gcol[:, cc:cc + 1], rstd, ALU.mult, ALU.mult, [b_hk[cc // 4], b_rstd], [b_uT[cc]])
        if tb == 0:
            k.dump("rstd", rstd, [b_rstd])
            k.dump("uT0", uT[:, 0, :], [b_uT[0]], BF16)
            k.dump("uT5", uT[:, 5, :], [b_uT[5]], BF16)
        c.barrier()
        for f in range(NFC):
            sl = f % 3
            pset = (f % 2) * 4
            if f >= 3:
                c.dma("pool", wgt[sl], wgu_v[:, :, f * 128:(f + 1) * 128], [], [b_wg[sl]], b_wg[sl])
                c.dma("pool", wut[sl], wgu_v[:, :, FF + f * 128:FF + (f + 1) * 128], [], [b_wu[sl]], b_wu[sl])
            for wi, (wt, bw) in enumerate(((wgt, b_wg), (wut, b_wu))):
                for hf in range(2):
                    bank = pset + wi * 2 + hf
                    for kc in range(16):
                        c.mm(ps[bank], wt[sl][:, kc, :], uT[:, kc, hf * 512:(hf + 1) * 512], kc == 0, kc == 15,
                             [bw[sl], b_uT[kc]], [b_ps[bank]], inc=(kc == 15))
            ss = f % 2
            for hf in range(2):
                c.act(sgt[ss][:, hf * 512:(hf + 1) * 512], ps[pset + hf], AF.Silu, [b_ps[pset + hf]], [b_sgt[ss]])
            for hf in range(2):
                c.tt("dve", actT[:, f, hf * 512:(hf + 1) * 512], sgt[ss][:, hf * 512:(hf + 1) * 512], ps[pset + 2 + hf], ALU.mult,
                     [b_sgt[ss], b_ps[pset + 2 + hf]], [b_act[f]])
        if tb == 0:
            k.dump("act0", actT[:, 0, :], [b_act[0]], BF16)
            k.dump("act7", actT[:, 7, :], [b_act[7]], BF16)
        c.barrier()
        for dc in range(16):
            sl = dc % 2
            for g0 in range(0, NFC, 11):
                g1 = min(NFC, g0 + 11)
                c.dma("pool", wdt[sl][:, g0:g1, :], wd_v[:, g0:g1, dc * 128:(dc + 1) * 128], [], [b_wd[sl]], b_wd[sl])
            c.dma("sp", hres[sl], src[dc * 128:(dc + 1) * 128, t0:t0 + NT], [k.b_h], [b_hres[sl]], b_hres[sl])
            pset = (dc % 4) * 2
            for hf in range(2):
                for fc in range(NFC):
                    c.mm(ps[pset + hf], wdt[sl][:, fc, :], actT[:, fc, hf * 512:(hf + 1) * 512], fc == 0, fc == NFC - 1,
                         [b_wd[sl], b_act[fc]], [b_ps[pset + hf]], inc=(fc == NFC - 1))
            for hf in range(2):
                c.stt(hres[sl][:, hf * 512:(hf + 1) * 512], ps[pset + hf], 0.5, hres[sl][:, hf * 512:(hf + 1) * 512],
                      ALU.mult, ALU.add, [b_ps[pset + hf], b_hres[sl]], [b_hres[sl]])
            c.dma("sp", dst[dc * 128:(dc + 1) * 128, t0:t0 + NT], hres[sl], [b_hres[sl]], [k.b_h], b_hst[sl])
        c.barrier()
    c.pop_scope()
    ar.reset(m0)


def build(plan, n_layers=NL):
    nc = bass.Bass("TRN2", target_bir_lowering=False)
    k = K()
    k.nc = nc
    dt = nc.dram_tensor
    xT = dt("xT", [D, S], F32, kind="ExternalInput").ap()
    cols_d = dt("cols", [128, NL * NCOLS], F32, kind="ExternalInput").ap()
    cst_d = dt("cst", [128, 3 * 128], F32, kind="ExternalInput").ap()
    tbraw_d = dt("tbraw", [128, 2 * 8 * 128], F32, kind="ExternalInput").ap()
    rb31_d = dt("rb31", [128, 8], F32, kind="ExternalInput").ap()
    k.w = {}
    k.w["ffn1_w_gu"] = dt("ffn1_w_gu", [NL, D, 2 * FF], F32, kind="ExternalInput").ap()
    k.w["ffn1_w_down"] = dt("ffn1_w_down", [NL, FF, D], F32, kind="ExternalInput").ap()
    k.w["w_in"] = dt("w_in", [NL, D, D_IN], F32, kind="ExternalInput").ap()
    k.w["w_att_out"] = dt("w_att_out", [NL, H * R, D], F32, kind="ExternalInput").ap()
    k.w["w_mem_out"] = dt("w_mem_out", [NL, MH * DV, D], F32, kind="ExternalInput").ap()
    k.w["w_out"] = dt("w_out", [NL, D, D], F32, kind="ExternalInput").ap()
    k.w["ffn2_w_gu"] = dt("ffn2_w_gu", [NL, D, 2 * FF], F32, kind="ExternalInput").ap()
    k.w["ffn2_w_down"] = dt("ffn2_w_down", [NL, FF, D], F32, kind="ExternalInput").ap()
    outT = dt("outT", [D, S], F32, kind="ExternalOutput").ap()
    k.xT, k.outT = xT, outT
    ikind = "ExternalOutput" if K.dbg_on else "Internal"
    k.hbuf = dt("hbuf", [D, S], F32, kind=ikind).ap()
    k.dbg = {}

    with ExitStack() as es:
        c = Ctx(nc, es)
        k.c = c
        ar = Arena(nc, es, 207 * 1024)
        k.ar = ar
        k.ps = []
        k.b_ps = []
        for i in range(8):
            t = es.enter_context(nc.psum_tensor("ps%d" % i, [128, 512], F32))
            k.ps.append(t[:, :])
            k.b_ps.append(c.buf("ps%d" % i))
        k.b_h = c.buf("hdram", multi=True)
        k.cols = ar.alloc([NL * NCOLS], F32)
        k.cst = ar.alloc([3 * 128], F32)
        k.ones_f = ar.alloc([128], F32)
        k.ones_b = ar.alloc([128], BF16)
        k.eps_col = ar.alloc([1], F32)
        k.ident_b = ar.alloc([128], BF16)
        k.ident4_b = ar.alloc([4, 128], BF16)
        k.tri_b = ar.alloc([128], BF16)
        k.ident_f = k.cst[:, 0:128]
        k.cneg_f = k.cst[:, 128:256]
        k.b_const = c.buf("const")
        b_ld = c.buf("cld")
        c.dma("sp", k.cols, cols_d, [], [k.b_const], b_ld)
        c.dma("sp", k.cst, cst_d, [], [k.b_const], b_ld)
        c.memset("dve", k.ones_f, 1.0, [k.b_const])
        c.memset("dve", k.ones_b, 1.0, [k.b_const])
        c.memset("dve", k.eps_col, EPS, [k.b_const])
        c.barrier()
        c.copy("dve", k.ident_b, k.cst[:, 0:128], [], [k.b_const])
        c.copy("dve", k.tri_b, k.cst[:, 256:384], [], [k.b_const])
        for i in range(4):
            c.copy("dve", k.ident4_b[:, i, :], k.cst[:, 0:128], [], [k.b_const])
        c.barrier()

        def col(l, cidx, n=1):
            return k.cols[:, l * NCOLS + cidx: l * NCOLS + cidx + n]
        k.col = col

        k.one_col = ar.alloc([1], F32)
        c.memset("dve", k.one_col, 1.0, [k.b_const])
        k.TB = ar.alloc([2, 8, 128], F32)
        k.b_TB = c.buf("TB")
        rb31 = ar.alloc([8], F32)
        c.dma("sp", k.TB.rearrange("p a b c -> p (a b c)"), tbraw_d, [], [k.b_TB], b_ld)
        c.dma("sp", rb31, rb31_d, [], [k.b_const], b_ld)
        for kind in range(2):
            for h in range(8):
                c.ts("dve", k.TB[:, kind, h, :], k.TB[:, kind, h, :], rb31[:, h:h + 1], None, ALU.subtract, None,
                     [k.b_TB, k.b_const], [k.b_TB])
        k.TBh = ar.alloc([2, 8, 128], BF16)
        k.TBl = ar.alloc([2, 8, 128], BF16)
        tbf = k.TB.rearrange("p a b c -> p (a b c)")
        c.copy("dve", k.TBh.rearrange("p a b c -> p (a b c)"), tbf, [k.b_TB], [k.b_TB])
        c.tt("dve", k.TBl.rearrange("p a b c -> p (a b c)"), tbf, k.TBh.rearrange("p a b c -> p (a b c)"), ALU.subtract,
             [k.b_TB], [k.b_TB])
        c.barrier()
        k.b_scr = c.buf("scr", multi=True)
        k.q_scr = dt("q_scr", [16, 128, 2, 8, 128], BF16, kind=ikind).ap()
        k.o_scr = dt("o_scr", [16, 128, S], BF16, kind=ikind).ap()
        k.mqk_scr = dt("mqk_scr", [8, 128, S], BF16, kind=ikind).ap()
        k.v_scr = dt("v_scr", [4, 128, 16, 256], BF16, kind=ikind).ap()
        k.sgo_scr = dt("sgo_scr", [8, 128, S], BF16, kind=ikind).ap()
        k.sga_scr = dt("sga_scr", [16, 128, S], BF16, kind=ikind).ap()
        k.sgm_scr = dt("sgm_scr", [16, 128, S], BF16, kind=ikind).ap()
        k.hm_scr = dt("hm_scr", [8, 128, S], BF16, kind=ikind).ap()
        mL = ar.mark()

        def alloc_mixer():
            ar.reset(mL)
            k.gi = ar.alloc([S], F32)
            k.gf = ar.alloc([S], F32)
            k.b_gi, k.b_gf = c.buf("gi"), c.buf("gf")
            k.mA = ar.mark()
            k.ckvT = ar.alloc([2, S], BF16)
            k.ckv = ar.alloc([16, 256], BF16)
            k.iqT = ar.alloc([4, S], BF16)
            k.ikT = ar.alloc([S], BF16)
            k.iw = ar.alloc([16, 8], F32)
            k.b_ckvT, k.b_ckv, k.b_iqT, k.b_ikT, k.b_iw = (c.buf(n) for n in ("ckvT", "ckv", "iqT", "ikT", "iw"))

        for st in plan:
            if st[0] == "m1":
                alloc_mixer()
                m1_stage(k, st[1])
            elif st[0] == "m2":
                m2_stage(k, st[1])
                ar.reset(k.mA)
            elif st[0] == "m3":
                m3_stage(k, st[1])
                ar.reset(mL)
            elif st[0] == "m4":
                m4_stage(k, st[1])
            elif st[0] == "copy":
                pass
            elif st[0] == "ffn":
                _, l, which, src, dst = st
                srcap = {"x": xT, "h": k.hbuf, "o": outT}[src]
                dstap = {"x": xT, "h": k.hbuf, "o": outT}[dst]
                wn = "ffn1" if which == 1 else "ffn2"
                ffn_stage(k, srcap, dstap, col(l, C_FFN1 if which == 1 else C_FFN2, 16),
                          k.w[wn + "_w_gu"][l], k.w[wn + "_w_down"][l])
            else:
                raise ValueError(st)
        c.barrier()
        c.emit()
    print("instructions ~", c.ninstr, "dma sems", c.nsem)
    return nc


def host_consts(rel_bias):
    ident = np.eye(128, dtype=np.float32)
    tl = np.arange(128)
    cneg = np.where(tl[None, :] <= tl[:, None], 0.0, -1e30).astype(np.float32)
    tri = (tl[None, :] >= tl[:, None]).astype(np.float32)
    cst = np.concatenate([ident, cneg, tri], axis=1)
    d = np.arange(256)
    n = np.maximum(d, 0)
    nl = np.maximum(n, 16).astype(np.float32)
    large = 16 + (np.log(nl / 16) / math.log(128 / 16) * 16).astype(np.int32)
    large = np.minimum(large, 31)
    bucket = np.where(n < 16, n, large)
    sl = np.arange(128)[:, None]
    tt = np.arange(128)[None, :]
    tb = np.zeros((128, 2, 8, 128), np.float32)
    for kind in range(2):
        dd = np.clip(tt - sl + 128 * kind, 0, 255)
        tb[:, kind, :, :] = np.transpose(rel_bias[bucket[dd]], (0, 2, 1))
    rb31 = np.broadcast_to(rel_bias[31][None, :], (128, 8)).copy()
    return cst, tb.reshape(128, -1), rb31


def host_cols(inp):
    cols = np.zeros((NL, 128, NCOLS), np.float32)
    for l in range(NL):
        cols[l, :, C_FFN1:C_FFN1 + 16] = inp["ffn1_norm"][l].reshape(16, 128).T
        cols[l, :, C_MIX:C_MIX + 16] = inp["mix_norm"][l].reshape(16, 128).T
        cols[l, :, C_FFN2:C_FFN2 + 16] = inp["ffn2_norm"][l].reshape(16, 128).T
        cols[l, :, C_QN:C_QN + 2] = inp["q_norm"][l].reshape(2, 128).T
        cols[l, :, C_KVN:C_KVN + 2] = inp["kv_norm"][l].reshape(2, 128).T
        cols[l, :, C_CONVW:C_CONVW + 32] = inp["conv_w"][l].reshape(4, 8, 128).transpose(2, 0, 1).reshape(128, 32)
        cols[l, :, C_CONVB:C_CONVB + 8] = inp["conv_b"][l].reshape(8, 128).T
        cols[l, :, C_MON:C_MON + 8] = inp["m_out_norm"][l].reshape(4, 2, 128).transpose(2, 0, 1).reshape(128, 8)
        cols[l, 0:4, C_IB] = inp["igate_b"][l]
        cols[l, 0:4, C_FB] = inp["fgate_b"][l]
    return np.ascontiguousarray(cols.transpose(1, 0, 2).reshape(128, NL * NCOLS))


def rms_block(k, src, t0, NT, gcol, uT, b_uT):
    c, ar = k.c, k.ar
    ps, b_ps = k.ps, k.b_ps
    m0 = ar.mark()
    c.push_scope()
    hld = [ar.alloc([NT], F32) for _ in range(2)]
    sqt = [ar.alloc([NT], F32) for _ in range(2)]
    rstd = ar.alloc([NT], F32)
    tmp = ar.alloc([NT], F32)
    b_hld = [c.buf("hld%d" % i) for i in range(2)]
    b_sqt = [c.buf("sqt%d" % i) for i in range(2)]
    b_rstd = c.buf("rstd")
    b_tmp = c.buf("tmp")
    nh = NT // 512
    for cc in range(16):
        sl = cc % 2
        c.dma("sp", hld[sl], src[cc * 128:(cc + 1) * 128, t0:t0 + NT], [k.b_h], [b_hld[sl]], b_hld[sl])
        c.act(sqt[sl], hld[sl], AF.Square, [b_hld[sl]], [b_sqt[sl]])
        for hf in range(nh):
            c.mm(ps[hf], k.ones_f, sqt[sl][:, hf * 512:(hf + 1) * 512], cc == 0, cc == 15,
                 [b_sqt[sl]], [b_ps[hf]], inc=(hf == nh - 1))
    for hf in range(nh):
        c.act(tmp[:, hf * 512:(hf + 1) * 512], ps[hf], AF.Ln, [b_ps[hf]], [b_tmp], bias=k.eps_col, scale=1.0 / D)
    c.act(rstd, tmp, AF.Exp, [b_tmp], [b_rstd], scale=-0.5)
    for cc in range(16):
        sl = cc % 2
        c.dma("sp", hld[sl], src[cc * 128:(cc + 1) * 128, t0:t0 + NT], [k.b_h], [b_hld[sl]], b_hld[sl])
        c.stt(uT[:, cc, :], hld[sl], gcol[:, cc:cc + 1], rstd, ALU.mult, ALU.mult, [b_hld[sl], b_rstd], [b_uT[cc]])
    c.barrier()
    c.pop_scope()
    ar.reset(m0)


def m1_stage(k, l):
    c, ar = k.c, k.ar
    ps, b_ps = k.ps, k.b_ps
    NT = 1024
    src = k.hbuf
    wv = k.w["w_in"][l].rearrange("(kc p) m -> p kc m", p=128)
    m0 = ar.mark()
    c.push_scope()
    uT = ar.alloc([16, NT], BF16)
    b_uT = [c.buf("uT%d" % i) for i in range(16)]
    wt = [ar.alloc([16, 128], BF16) for _ in range(3)]
    b_wt = [c.buf("wt%d" % i) for i in range(3)]
    raw = [ar.alloc([NT], F32) for _ in range(2)]
    b_raw = [c.buf("raw%d" % i) for i in range(2)]
    sqq = [ar.alloc([NT], BF16) for _ in range(2)]
    b_sqq = [c.buf("sqq%d" % i) for i in range(2)]
    tmpn = ar.alloc([NT], F32)
    b_tmpn = c.buf("tmpn")
    rinv = ar.alloc([NT], F32)
    b_rinv = c.buf("rinv")
    stg = [ar.alloc([NT], BF16) for _ in range(3)]
    b_stg = [c.buf("stg%d" % i) for i in range(3)]
    b_sst = [c.buf("sst%d" % i) for i in range(3)]
    rawc = [ar.alloc([NT + 8], F32) for _ in range(2)]
    b_rawc = [c.buf("rawc%d" % i) for i in range(2)]
    acc = [ar.alloc([NT], F32) for _ in range(2)]
    b_acc = [c.buf("acc%d" % i) for i in range(2)]
    halo = ar.alloc([8, 4], F32)
    b_halo = c.buf("halo")
    iwrow = ar.alloc([NT], F32)
    b_iwrow = c.buf("iwrow")
    vst = [ar.alloc([8, 128], BF16) for _ in range(2)]
    b_vst = [c.buf("vst%d" % i) for i in range(2)]
    b_vss = [c.buf("vss%d" % i) for i in range(2)]
    gqs = ar.alloc([2], F32)
    b_gqs = c.buf("gqs")
    c.ts("dve", gqs, k.col(l, C_QN, 2), R ** -0.5, None, ALU.mult, None, [k.b_const], [b_gqs])
    ps6b = ps[6].bitcast(BF16)
    st = {"w": 0, "p": 0, "s": 0}

    pending = []

    def prefetch(col0, width):
        sl = (st["w"] + len(pending)) % 3
        c.dma("pool", wt[sl][:, :, 0:width], wv[:, :, col0:col0 + width], [], [b_wt[sl]], b_wt[sl])
        pending.append((col0, width, sl))

    def proj(col0, width, dup=False):
        sl = st["w"] % 3
        st["w"] += 1
        bank0 = (st["p"] % 2) * 2
        st["p"] += 1
        if pending:
            pc, pw_, psl = pending.pop(0)
            assert (pc, pw_, psl) == (col0, width, sl) and not dup
        elif dup:
            c.dma("pool", wt[sl][:, :, 0:64], wv[:, :, col0:col0 + 64], [], [b_wt[sl]], b_wt[sl])
            c.dma("pool", wt[sl][:, :, 64:128], wv[:, :, col0:col0 + 64], [], [b_wt[sl]], b_wt[sl])
            width = 128
        else:
            c.dma("pool", wt[sl][:, :, 0:width], wv[:, :, col0:col0 + width], [], [b_wt[sl]], b_wt[sl])
        for hf in range(2):
            for kc in range(16):
                c.mm(ps[bank0 + hf][0:width, :], wt[sl][:, kc, 0:width], uT[:, kc, hf * 512:(hf + 1) * 512], kc == 0, kc == 15,
                     [b_wt[sl], b_uT[kc]], [b_ps[bank0 + hf]], inc=(kc == 15))
        return bank0

    def stage_slot():
        s_ = st["s"] % 3
        st["s"] += 1
        return s_

    import os
    SEC = os.environ.get("M1_SEC", "q,iq,ik,iw,mqk,mv,gates,gi").split(",")
    for th in range(2):
        t0 = th * NT
        if "q" in SEC:
            for i_ in range(3):
                prefetch(OFF_AQ + i_ * 128, 128)
        rms_block(k, src, t0, NT, k.col(l, C_MIX, 16), uT, b_uT)
        for g in (range(9) if "q" in SEC else []):
            gcols = gqs if g < 8 else k.col(l, C_KVN, 2)
            b_gc = b_gqs if g < 8 else k.b_const
            for rc in range(2):
                bank0 = proj(OFF_AQ + g * 256 + rc * 128, 128)
                for hf in range(2):
                    c.copy("dve", raw[rc][:, hf * 512:(hf + 1) * 512], ps[bank0 + hf], [b_ps[bank0 + hf]], [b_raw[rc]])
                    if not os.environ.get("NOSQ"):
                        c.act(sqq[rc][:, hf * 512:(hf + 1) * 512], raw[rc][:, hf * 512:(hf + 1) * 512], AF.Square, [b_raw[rc]], [b_sqq[rc]])
                for hf in range(2):
                    if not os.environ.get("NOSSQ"):
                        c.mm(ps[4 + hf], k.ones_b, sqq[rc][:, hf * 512:(hf + 1) * 512], rc == 0, rc == 1,
                             [b_sqq[rc]], [b_ps[4 + hf]], inc=True)
            for hf in range(2):
                c.act(tmpn[:, hf * 512:(hf + 1) * 512], ps[4 + hf], AF.Ln, [b_ps[4 + hf]], [b_tmpn], bias=k.eps_col, scale=1.0 / R)
            c.act(rinv, tmpn, AF.Exp, [b_tmpn], [b_rinv], scale=-0.5)
            for rc in range(2):
                if g < 8:
                    ss = stage_slot()
                    c.stt(stg[ss], raw[rc], gcols[:, rc:rc + 1], rinv, ALU.mult, ALU.mult, [b_raw[rc], b_rinv, b_gc], [b_stg[ss]])
                    dstq = k.q_scr[th * 8:(th + 1) * 8, :, rc, g, :].rearrange("j p t -> p j t")
                    if not os.environ.get("NOQSTORE"):
                        c.dma("sp", dstq, stg[ss].rearrange("p (j t) -> p j t", j=8), [b_stg[ss]], [k.b_scr], b_sst[ss])
                else:
                    c.stt(k.ckvT[:, rc, t0:t0 + NT], raw[rc], gcols[:, rc:rc + 1], rinv, ALU.mult, ALU.mult,
                          [b_raw[rc], b_rinv, b_gc], [k.b_ckvT])
            if g == 8 and not os.environ.get("NOCKVTR"):
                for grp in range(2):
                    for tl in range(4):
                        tile_ = th * 8 + grp * 4 + tl
                        for rc in range(2):
                            c.tr(ps6b[:, (tl * 2 + rc) * 128:(tl * 2 + rc + 1) * 128], k.ckvT[:, rc, tile_ * 128:(tile_ + 1) * 128],
                                 k.ident_b, [k.b_ckvT, k.b_const], [b_ps[6]], inc=(tl == 3 and rc == 1))
                    c.copy("dve", k.ckv[:, th * 8 + grp * 4:th * 8 + grp * 4 + 4, :].rearrange("p a b -> p (a b)"), ps6b,
                           [b_ps[6]], [k.b_ckv])
        for cc in (range(4) if "iq" in SEC else []):
            bank0 = proj(OFF_IQ + cc * 128, 128)
            for hf in range(2):
                c.copy("act", k.iqT[:, cc, t0 + hf * 512:t0 + (hf + 1) * 512], ps[bank0 + hf], [b_ps[bank0 + hf]], [k.b_iqT])
        if "ik" in SEC:
            bank0 = proj(OFF_IK, 64, dup=True)
            for hf in range(2):
                c.copy("act", k.ikT[:, t0 + hf * 512:t0 + (hf + 1) * 512], ps[bank0 + hf], [b_ps[bank0 + hf]], [k.b_ikT])
        bank0 = proj(OFF_IW, 8) if "iw" in SEC else 0
        for hf in (range(2) if "iw" in SEC else []):
            c.act(iwrow[0:8, hf * 512:(hf + 1) * 512], ps[bank0 + hf][0:8, :], AF.Copy, [b_ps[bank0 + hf]], [b_iwrow],
                  scale=float((HI * DI) ** -0.5))
        for tl in (range(8) if "iw" in SEC else []):
            c.tr(ps[7][:, tl * 8:(tl + 1) * 8], iwrow[0:8, tl * 128:(tl + 1) * 128], k.ident_f[0:8, 0:8],
                 [b_iwrow, k.b_const], [b_ps[7]], inc=(tl == 7))
        if "iw" in SEC:
            c.copy("dve", k.iw[:, th * 8:(th + 1) * 8, :].rearrange("p a b -> p (a b)"), ps[7][:, 0:64], [b_ps[7]], [k.b_iw])
        for cc in (range(8) if "mqk" in SEC else []):
            bank0 = proj(OFF_MQK + cc * 128, 128)
            rs = cc % 2
            for hf in range(2):
                c.copy("act", rawc[rs][:, 3 + hf * 512:3 + (hf + 1) * 512], ps[bank0 + hf], [b_ps[bank0 + hf]], [b_rawc[rs]])
            if th == 0:
                c.memset("dve", rawc[rs][:, 0:3], 0.0, [b_rawc[rs]])
            else:
                c.copy("dve", rawc[rs][:, 0:3], halo[:, cc, 0:3], [b_halo], [b_rawc[rs]])
            cw = k.col(l, C_CONVW, 32)
            c.ts("dve", acc[rs], rawc[rs][:, 0:NT], cw[:, cc:cc + 1], None, ALU.mult, None, [b_rawc[rs], k.b_const], [b_acc[rs]])
            for w_ in range(1, 4):
                c.stt(acc[rs], rawc[rs][:, w_:w_ + NT], cw[:, w_ * 8 + cc:w_ * 8 + cc + 1], acc[rs], ALU.mult, ALU.add,
                      [b_rawc[rs], b_acc[rs], k.b_const], [b_acc[rs]])
            if th == 0:
                c.copy("dve", halo[:, cc, 0:3], rawc[rs][:, NT:NT + 3], [b_rawc[rs]], [b_halo])
            ss = stage_slot()
            c.act(stg[ss], acc[rs], AF.Silu, [b_acc[rs], k.b_const], [b_stg[ss]], bias=k.col(l, C_CONVB + cc, 1))
            c.dma("sp", k.mqk_scr[cc, :, t0:t0 + NT], stg[ss], [b_stg[ss]], [k.b_scr], b_sst[ss])
        for cc in (range(8) if "mv" in SEC else []):
            bank0 = proj(OFF_MV + cc * 128, 128)
            ss = stage_slot()
            for hf in range(2):
                c.copy("act", stg[ss][:, hf * 512:(hf + 1) * 512], ps[bank0 + hf], [b_ps[bank0 + hf]], [b_stg[ss]])
            for tl in range(8):
                c.tr(ps6b[:, tl * 128:(tl + 1) * 128], stg[ss][:, tl * 128:(tl + 1) * 128], k.ident_b,
                     [b_stg[ss], k.b_const], [b_ps[6]], inc=(tl == 7))
            vs = cc % 2
            c.copy("dve", vst[vs].rearrange("p a b -> p (a b)"), ps6b, [b_ps[6]], [b_vst[vs]])
            c.dma("sp", k.v_scr[cc // 2, :, th * 8:(th + 1) * 8, (cc % 2) * 128:(cc % 2 + 1) * 128], vst[vs],
                  [b_vst[vs]], [k.b_scr], b_vss[vs])
        for (off, n, scr) in (((OFF_MO, 8, k.sgo_scr), (OFF_GA, 16, k.sga_scr), (OFF_GM, 16, k.sgm_scr)) if "gates" in SEC else []):
            for cc in range(n):
                bank0 = proj(off + cc * 128, 128)
                ss = stage_slot()
                for hf in range(2):
                    c.act(stg[ss][:, hf * 512:(hf + 1) * 512], ps[bank0 + hf], AF.Sigmoid, [b_ps[bank0 + hf]], [b_stg[ss]])
                c.dma("sp", scr[cc, :, t0:t0 + NT], stg[ss], [b_stg[ss]], [k.b_scr], b_sst[ss])
        for (off, cb, dstrow, bb) in (((OFF_MI, C_IB, k.gi, k.b_gi), (OFF_MF, C_FB, k.gf, k.b_gf)) if "gi" in SEC else []):
            bank0 = proj(off, 4)
            for hf in range(2):
                c.act(dstrow[0:4, t0 + hf * 512:t0 + (hf + 1) * 512], ps[bank0 + hf][0:4, :], AF.Identity,
                      [b_ps[bank0 + hf], k.b_const], [bb], bias=k.col(l, cb, 1)[0:4, :])
        c.barrier()
    c.pop_scope()
    ar.reset(m0)


def m2_stage(k, l):
    c, ar = k.c, k.ar
    ps, b_ps = k.ps, k.b_ps
    m0 = ar.mark()
    c.push_scope()
    NS = 4
    qj = [ar.alloc([2, 8, 128], BF16) for _ in range(NS)]
    b_qj = [c.buf("qj%d" % i) for i in range(NS)]
    sc = [ar.alloc([S], F32) for _ in range(NS)]
    b_sc = [c.buf("sc%d" % i) for i in range(NS)]
    NIT = 24
    junk = [ar.alloc([S], BF16) for _ in range(2)]
    b_junk = [c.buf("junk%d" % i) for i in range(2)]
    lo = [ar.alloc([1], F32) for _ in range(2)]
    w0 = [ar.alloc([1], F32) for _ in range(2)]
    midc = [ar.alloc([1], F32) for _ in range(2)]
    cntc = [ar.alloc([1], F32) for _ in range(2)]
    tmpc = [ar.alloc([1], F32) for _ in range(2)]
    Wb = [ar.alloc([NIT + 1], F32) for _ in range(2)]
    nW = [ar.alloc([NIT + 1], F32) for _ in range(2)]
    m8 = [ar.alloc([8], F32) for _ in range(2)]
    pw = ar.alloc([NIT + 1], F32)
    b_bis = [c.buf("bis%d" % i) for i in range(2)]
    b_pw = c.buf("pw")
    for n_ in range(NIT + 1):
        c.memset("dve", pw[:, n_:n_ + 1], 2.0 ** -(n_ + 1), [b_pw])
    thr0 = ar.alloc([1], F32)
    rl = [ar.alloc([512], BF16) for _ in range(4)]
    b_rl = [c.buf("rl%d" % i) for i in range(4)]
    dg = [ar.alloc([8, 128], BF16) for _ in range(NS)]
    b_dg = [c.buf("dg%d" % i) for i in range(NS)]
    negm = [ar.alloc([S], BF16) for _ in range(NS)]
    b_negm = [c.buf("negm%d" % i) for i in range(NS)]
    pT = [ar.alloc([512], BF16) for _ in range(3)]
    b_pT = [c.buf("pT%d" % i) for i in range(3)]
    oraw = [ar.alloc([2, 512], F32) for _ in range(2)]
    b_oraw = [c.buf("oraw%d" % i) for i in range(2)]
    rcp = [ar.alloc([512], F32) for _ in range(2)]
    b_rcp = [c.buf("rcp%d" % i) for i in range(2)]
    ost = [ar.alloc([4, 128], BF16) for _ in range(4)]
    b_ost = [c.buf("ost%d" % i) for i in range(4)]
    b_oss = [c.buf("oss%d" % i) for i in range(4)]
    b_thr0 = c.buf("thr0")
    c.memset("dve", thr0, -1e29, [b_thr0])
    o_v = k.o_scr.rearrange("(h rc) p t -> rc p h t", rc=2)
    id4 = k.ident4_b.rearrange("p a b -> p (a b)")
    cnt = {"idx": 0, "lg": 0, "pt": 0, "os": 0, "hg": 0}

    def scores(j):
        SP = (j + 1) * 128
        qs = j % NS
        c.dma("sp", qj[qs], k.q_scr[j], [k.b_scr], [b_qj[qs]], b_qj[qs])
        scj = sc[qs]
        b_scj = b_sc[qs]
        for h in range(HI):
            c.ts("pool", dg[qs][:, h, :], k.ident_f, k.iw[:, j, h:h + 1], None, ALU.mult, None, [k.b_const, k.b_iw], [b_dg[qs]])
        nblk = (SP + 511) // 512
        for blk in range(nblk):
            w_ = min(512, SP - blk * 512)
            for h in range(HI):
                ch, pb = h // 2, (h % 2) * 64
                ib = cnt["idx"] % 2
                rs = cnt["idx"] % 4
                cnt["idx"] += 1
                c.mm(ps[ib][:, 0:w_], k.iqT[pb:pb + 64, ch, j * 128:(j + 1) * 128], k.ikT[pb:pb + 64, blk * 512:blk * 512 + w_],
                     True, True, [k.b_iqT, k.b_ikT], [b_ps[ib]])
                c.act(rl[rs][:, 0:w_], ps[ib][:, 0:w_], AF.Relu, [b_ps[ib]], [b_rl[rs]])
                c.mm(ps[7][:, 0:w_], dg[qs][:, h, :], rl[rs][:, 0:w_], h == 0, h == HI - 1, [b_dg[qs], b_rl[rs]], [b_ps[7]])
            c.copy("act", scj[:, blk * 512:blk * 512 + w_], ps[7][:, 0:w_], [b_ps[7]], [b_scj])
        c.tt("dve", scj[:, j * 128:(j + 1) * 128], scj[:, j * 128:(j + 1) * 128], k.cneg_f, ALU.add, [b_scj, k.b_const], [b_scj])

    def bisect(js):
        js = [j for j in js if j >= 2]
        for j in js:
            x = j % 2
            qs = j % NS
            SP = (j + 1) * 128
            sct = sc[qs][:, 0:SP]
            c.op("dve", (lambda o, i_: (lambda e: e.max(out=o, in_=i_)))(m8[x], sct), reads=[b_sc[qs]], writes=[b_bis[x]])
            c.op("dve", (lambda o, i_: (lambda e: e.tensor_reduce(out=o, in_=i_, axis=AX.X, op=ALU.min)))(lo[x], sc[qs][:, 0:j * 128]),
                 reads=[b_sc[qs]], writes=[b_bis[x]])
            c.tt("dve", w0[x], m8[x][:, 0:1], lo[x], ALU.subtract, [b_bis[x]], [b_bis[x]])
            c.ts("dve", Wb[x], pw, w0[x], None, ALU.mult, None, [b_bis[x], b_pw], [b_bis[x]])
            c.ts("dve", nW[x], Wb[x], -1.0, None, ALU.mult, None, [b_bis[x]], [b_bis[x]])
            c.tt("dve", midc[x], lo[x], Wb[x][:, 0:1], ALU.add, [b_bis[x]], [b_bis[x]])
        for n_ in range(NIT):
            for j in js:
                x = j % 2
                qs = j % NS
                SP = (j + 1) * 128
                c.op("dve", (lambda o, i_, m_, a_: (lambda e: e.tensor_scalar(out=o, in0=i_, scalar1=m_, scalar2=0.0, op0=ALU.is_ge,
                                                                             op1=ALU.add, accum_out=a_)))(
                    junk[x][:, 0:SP], sc[qs][:, 0:SP], midc[x], cntc[x]), reads=[b_sc[qs], b_bis[x]], writes=[b_bis[x], b_junk[x]])
            for j in js:
                x = j % 2
                c.ts("dve", tmpc[x], cntc[x], float(TOPK) - 0.5, Wb[x][:, n_:n_ + 1], ALU.is_ge, ALU.mult, [b_bis[x]], [b_bis[x]])
                c.stt(midc[x], tmpc[x], nW[x][:, n_ + 1:n_ + 2], midc[x], ALU.add, ALU.add, [b_bis[x]], [b_bis[x]])
        for j in js:
            x = j % 2
            c.tt("dve", lo[x], midc[x], nW[x][:, NIT:NIT + 1], ALU.add, [b_bis[x]], [b_bis[x]])

    def negmask(j):
        SP = (j + 1) * 128
        qs = j % NS
        if j >= 2:
            thr, b_thr = lo[j % 2], b_bis[j % 2]
        else:
            thr, b_thr = thr0, b_thr0
        c.ts("dve", negm[qs][:, 0:SP], sc[qs][:, 0:SP], thr, NEG, ALU.is_lt, ALU.mult, [b_sc[qs], b_thr], [b_negm[qs]])

    def prep_pair(m):
        js = (2 * m, 2 * m + 1)
        for j in js:
            scores(j)
        bisect(js)
        for j in js:
            negmask(j)

    def attn(j):
        qs = j % NS
        nm = negm[qs]
        b_nm = b_negm[qs]
        for hg in range(2):
            lbs = {}

            def logits(i):
                lb = 2 + cnt["lg"] % 2
                cnt["lg"] += 1
                lbs[i] = lb
                for rc in range(2):
                    c.mm(ps[lb], k.ckvT[:, rc, i * 128:(i + 1) * 128], qj[qs][:, rc, hg * 4:(hg + 1) * 4, :].rearrange("p a b -> p (a b)"),
                         rc == 0, False, [k.b_ckvT, b_qj[qs]], [b_ps[lb]], inc=False)
                near = (j - i <= 1)
                if near:
                    for tbx in (k.TBh, k.TBl):
                        c.mm(ps[lb], k.ident_b, tbx[:, j - i, hg * 4:(hg + 1) * 4, :].rearrange("p a b -> p (a b)"), False, False,
                             [k.b_TB, k.b_const], [b_ps[lb]], inc=False)
                c.mm(ps[lb], nm[:, i * 128:(i + 1) * 128], id4, False, True, [b_nm, k.b_const], [b_ps[lb]])

            logits(0)
            for i in range(j + 1):
                if i + 1 <= j:
                    logits(i + 1)
                lb = lbs[i]
                pt = cnt["pt"] % 3
                cnt["pt"] += 1
                c.act(pT[pt], ps[lb], AF.Exp, [b_ps[lb]], [b_pT[pt]])
                for rc in range(2):
                    c.mm(ps[4 + rc], k.ckv[:, i, rc * 128:(rc + 1) * 128], pT[pt], i == 0, i == j, [k.b_ckv, b_pT[pt]], [b_ps[4 + rc]],
                         inc=False)
                c.mm(ps[6], k.ones_b, pT[pt], i == 0, i == j, [k.b_const, b_pT[pt]], [b_ps[6]])
            hs = cnt["hg"] % 2
            cnt["hg"] += 1
            for rc in range(2):
                c.copy("act", oraw[hs][:, rc, :], ps[4 + rc], [b_ps[4 + rc]], [b_oraw[hs]])
            c.act(rcp[hs], ps[6], AF.Ln, [b_ps[6]], [b_rcp[hs]])
            c.act(rcp[hs], rcp[hs], AF.Exp, [b_rcp[hs]], [b_rcp[hs]], scale=-1.0)
            for rc in range(2):
                os_ = cnt["os"] % 4
                cnt["os"] += 1
                c.tt("pool", ost[os_].rearrange("p a b -> p (a b)"), oraw[hs][:, rc, :], rcp[hs], ALU.mult, [b_oraw[hs], b_rcp[hs]],
                     [b_ost[os_]])
                c.dma("sp", o_v[rc, :, hg * 4:(hg + 1) * 4, j * 128:(j + 1) * 128], ost[os_], [b_ost[os_]], [k.b_scr], b_oss[os_])

    prep_pair(0)
    for m in range(8):
        if m + 1 < 8:
            prep_pair(m + 1)
        attn(2 * m)
        attn(2 * m + 1)
    c.barrier()
    c.pop_scope()
    ar.reset(m0)


def m3_stage(k, l):
    c, ar = k.c, k.ar
    ps, b_ps = k.ps, k.b_ps
    m0 = ar.mark()
    c.push_scope()
    e1 = ar.alloc([S], F32)
    l1 = ar.alloc([S], F32)
    Lc = ar.alloc([S], F32)
    a_ = e1
    M_ = l1
    nM = ar.alloc([S], F32)
    cl = ar.alloc([S], F32)
    zer = cl
    b_rows = c.buf("rows")
    Rh = ar.alloc([S], F32)
    Qh = ar.alloc([S], F32)
    Ch = ar.alloc([S], F32)
    b_Rh, b_Qh, b_Ch = c.buf("Rh"), c.buf("Qh"), c.buf("Ch")
    trineg = ar.alloc([128], F32)
    b_trineg = c.buf("trineg")
    acol = ar.alloc([16], F32)
    b_acol = c.buf("acol")
    nmbc = [ar.alloc([512], F32) for _ in range(2)]
    b_nmbc = [c.buf("nmbc%d" % i) for i in range(2)]
    qh = ar.alloc([S], BF16)
    kh = ar.alloc([S], BF16)
    vh = ar.alloc([16, 256], BF16)
    b_qh, b_kh, b_vh = c.buf("qh"), c.buf("kh"), c.buf("vh")
    clbc = ar.alloc([S], F32)
    b_clbc = c.buf("clbc")
    numT = ar.alloc([2, S], F32)
    b_numT = c.buf("numT")
    den = ar.alloc([S], F32)
    b_den = c.buf("den")
    ex = [ar.alloc([512], F32) for _ in range(2)]
    b_ex = [c.buf("ex%d" % i) for i in range(2)]
    dtl = [ar.alloc([512], F32) for _ in range(2)]
    b_dtl = [c.buf("dtl%d" % i) for i in range(2)]
    ptl = [ar.alloc([512], BF16) for _ in range(3)]
    b_ptl = [c.buf("ptl%d" % i) for i in range(3)]
    maskv = [ar.alloc([512], F32) for _ in range(4)]
    sq2 = ar.alloc([S], F32)
    b_sq2 = c.buf("sq2")
    rn, b_rn = clbc, b_clbc
    sgo = [ar.alloc([S], BF16) for _ in range(2)]
    b_sgo = [c.buf("sgo%d" % i) for i in range(2)]
    hst = [ar.alloc([S], BF16) for _ in range(2)]
    b_hst = [c.buf("hmst%d" % i) for i in range(2)]
    b_hss = [c.buf("hmss%d" % i) for i in range(2)]
    R4 = slice(0, 4)
    c.act(e1[R4, :], k.gf[R4, :], AF.Exp, [k.b_gf], [b_rows], scale=-1.0)
    c.act(l1[R4, :], e1[R4, :], AF.Ln, [b_rows], [b_rows], bias=k.one_col[R4, :])
    c.memset("dve", zer[0:4, :], 0.0, [b_rows])
    c.op("dve", (lambda o, d0, d1: (lambda e: e.tensor_tensor_scan(out=o, data0=d0, data1=d1, initial=0.0, op0=ALU.add, op1=ALU.add)))(
        Lc[R4, :], l1[R4, :], zer[R4, :]), reads=[b_rows], writes=[b_rows])
    c.tt("dve", a_[R4, :], k.gi[R4, :], Lc[R4, :], ALU.add, [k.b_gi, b_rows], [b_rows])
    c.op("dve", (lambda o, d0, d1: (lambda e: e.tensor_tensor_scan(out=o, data0=d0, data1=d1, initial=0.0, op0=ALU.max, op1=ALU.max)))(
        M_[R4, :], a_[R4, :], a_[R4, :]), reads=[b_rows], writes=[b_rows])
    c.ts("dve", nM[R4, :], M_[R4, :], -1.0, None, ALU.mult, None, [b_rows], [b_rows])
    c.tt("dve", cl[R4, :], Lc[R4, :], M_[R4, :], ALU.subtract, [b_rows], [b_rows])
    c.act(cl[R4, :], cl[R4, :], AF.Exp, [b_rows], [b_rows])
    c.ts("dve", trineg, k.cst[:, 256:384], -1.0, -NEG, ALU.add, ALU.mult, [k.b_const], [b_trineg])
    for d_ in range(4):
        c.memset("dve", maskv[d_], 0.0, [b_trineg])
        if d_ > 0:
            c.memset("dve", maskv[d_][:, 0:d_ * 128], NEG, [b_trineg])
        c.copy("dve", maskv[d_][:, d_ * 128:(d_ + 1) * 128], trineg, [b_trineg], [b_trineg])
    hm_v = k.hm_scr
    for h in range(MH):
        c.dma("sp", Rh[0:1, :], a_[h:h + 1, :], [b_rows], [b_Rh], b_Rh)
        c.dma("sp", Qh[0:1, :], nM[h:h + 1, :], [b_rows], [b_Qh], b_Qh)
        for i in range(16):
            c.tr(ps[7][:, i:i + 1], Rh[0:1, i * 128:(i + 1) * 128], k.ident_f[0:1, 0:1], [b_Rh, k.b_const], [b_ps[7]], inc=(i == 15))
        c.copy("dve", acol, ps[7][:, 0:16], [b_ps[7]], [b_acol])
        c.dma("sp", Ch[0:1, :], cl[h:h + 1, :], [b_rows], [b_Ch], b_Ch)
        c.dma("sp", qh, k.mqk_scr[h], [k.b_scr], [b_qh], b_qh)
        c.dma("sp", kh, k.mqk_scr[4 + h], [k.b_scr], [b_kh], b_kh)
        c.dma("sp", vh, k.v_scr[h], [k.b_scr], [b_vh], b_vh)
        for blk in range(4):
            c.mm(ps[7], k.ones_f[0:1, :], Ch[0:1, blk * 512:(blk + 1) * 512], True, True, [b_Ch, k.b_const], [b_ps[7]])
            c.copy("act", clbc[:, blk * 512:(blk + 1) * 512], ps[7], [b_ps[7]], [b_clbc])
        cnt = 0
        for tb in range(4):
            ni = 4 * tb + 4
            ts_ = slice(tb * 512, (tb + 1) * 512)
            slots = {}
            c.mm(ps[2], k.ones_f[0:1, :], Qh[0:1, ts_], True, True, [b_Qh, k.b_const], [b_ps[2]])
            c.copy("act", nmbc[tb % 2], ps[2], [b_ps[2]], [b_nmbc[tb % 2]])

            def front(i):
                nonlocal cnt
                sb = cnt % 2
                pt = cnt % 3
                cnt += 1
                slots[i] = pt
                c.mm(ps[sb], kh[:, i * 128:(i + 1) * 128], qh[:, ts_], True, True, [b_kh, b_qh], [b_ps[sb]])
                if i >= 4 * tb:
                    c.tt("dve", ex[sb], nmbc[tb % 2], maskv[i - 4 * tb], ALU.add, [b_nmbc[tb % 2], b_trineg], [b_ex[sb]])
                    c.act(dtl[sb], ex[sb], AF.Exp, [b_ex[sb], b_acol], [b_dtl[sb]], bias=acol[:, i:i + 1])
                else:
                    c.act(dtl[sb], nmbc[tb % 2], AF.Exp, [b_nmbc[tb % 2], b_acol], [b_dtl[sb]], bias=acol[:, i:i + 1])
                c.stt(ptl[pt], ps[sb], float(DK ** -0.5), dtl[sb], ALU.mult, ALU.mult, [b_ps[sb], b_dtl[sb]], [b_ptl[pt]])

            front(0)
            for i in range(ni):
                if i + 1 < ni:
                    front(i + 1)
                pt = slots[i]
                for cc in range(2):
                    c.mm(ps[4 + cc], vh[:, i, cc * 128:(cc + 1) * 128], ptl[pt], i == 0, i == ni - 1, [b_vh, b_ptl[pt]],
                         [b_ps[4 + cc]], inc=False)
                c.mm(ps[6], k.ones_b, ptl[pt], i == 0, i == ni - 1, [k.b_const, b_ptl[pt]], [b_ps[6]])
            for cc in range(2):
                c.copy("act", numT[:, cc, ts_], ps[4 + cc], [b_ps[4 + cc]], [b_numT])
            c.copy("act", den[:, ts_], ps[6], [b_ps[6]], [b_den])
        c.ts("dve", sq2, den, -1.0, None, ALU.mult, None, [b_den], [b_sq2])
        c.tt("dve", den, den, sq2, ALU.max, [b_den, b_sq2], [b_den])
        c.tt("dve", den, den, clbc, ALU.max, [b_den, b_clbc], [b_den])
        c.act(den, den, AF.Ln, [b_den], [b_den])
        c.act(den, den, AF.Exp, [b_den], [b_den], scale=-1.0)
        for cc in range(2):
            c.tt("dve", numT[:, cc, :], numT[:, cc, :], den, ALU.mult, [b_numT, b_den], [b_numT])
        for cc in range(2):
            c.act(sq2, numT[:, cc, :], AF.Square, [b_numT], [b_sq2])
            for blk in range(4):
                c.mm(ps[blk], k.ones_f, sq2[:, blk * 512:(blk + 1) * 512], cc == 0, cc == 1, [b_sq2, k.b_const], [b_ps[blk]])
        for blk in range(4):
            c.act(rn[:, blk * 512:(blk + 1) * 512], ps[blk], AF.Ln, [b_ps[blk]], [b_rn], bias=k.eps_col, scale=1.0 / DV)
        c.act(rn, rn, AF.Exp, [b_rn], [b_rn], scale=-0.5)
        for cc in range(2):
            ch = h * 2 + cc
            s_ = ch % 2
            c.dma("sp", sgo[s_], k.sgo_scr[ch], [k.b_scr], [b_sgo[s_]], b_sgo[s_])
            c.stt(numT[:, cc, :], numT[:, cc, :], k.col(l, C_MON + ch, 1), rn, ALU.mult, ALU.mult, [b_numT, b_rn, k.b_const], [b_numT])
            c.tt("dve", hst[s_], numT[:, cc, :], sgo[s_], ALU.mult, [b_numT, b_sgo[s_]], [b_hst[s_]])
            c.dma("sp", hm_v[ch], hst[s_], [b_hst[s_]], [k.b_scr], b_hss[s_])
    c.barrier()
    c.pop_scope()
    ar.reset(m0)


def m4_stage(k, l):
    c, ar = k.c, k.ar
    ps, b_ps = k.ps, k.b_ps
    NT = 1024
    m0 = ar.mark()
    c.push_scope()
    xatt = ar.alloc([16, NT], BF16)
    xmem = ar.alloc([8, NT], BF16)
    mrg = ar.alloc([16, NT], BF16)
    b_xatt, b_xmem = c.buf("xatt"), c.buf("xmem")
    b_mrg = [c.buf("mrg%d" % i) for i in range(16)]
    wa = [ar.alloc([16, 128], BF16) for _ in range(2)]
    wm = [ar.alloc([8, 128], BF16) for _ in range(2)]
    wo = [ar.alloc([16, 128], BF16) for _ in range(2)]
    b_wa = [c.buf("wa%d" % i) for i in range(2)]
    b_wm = [c.buf("wm%d" % i) for i in range(2)]
    b_wo = [c.buf("wo%d" % i) for i in range(2)]
    sga = [ar.alloc([NT], BF16) for _ in range(2)]
    sgm = [ar.alloc([NT], BF16) for _ in range(2)]
    b_sga = [c.buf("sga%d" % i) for i in range(2)]
    b_sgm = [c.buf("sgm%d" % i) for i in range(2)]
    t1 = [ar.alloc([NT], F32) for _ in range(2)]
    b_t1 = [c.buf("t1%d" % i) for i in range(2)]
    t2 = [ar.alloc([NT], F32) for _ in range(2)]
    b_t2 = [c.buf("t2%d" % i) for i in range(2)]
    hres = [ar.alloc([NT], F32) for _ in range(2)]
    b_hres = [c.buf("hres%d" % i) for i in range(2)]
    b_hst = [c.buf("hst%d" % i) for i in range(2)]
    wav = k.w["w_att_out"][l].rearrange("(kc p) m -> p kc m", p=128)
    wmv = k.w["w_mem_out"][l].rearrange("(kc p) m -> p kc m", p=128)
    wov = k.w["w_out"][l].rearrange("(kc p) m -> p kc m", p=128)
    o_in = k.o_scr.rearrange("c p t -> p c t")
    hm_in = k.hm_scr.rearrange("c p t -> p c t")
    for th in range(2):
        t0 = th * NT
        for dc in range(2):
            c.dma("pool", wo[dc], wov[:, :, dc * 128:(dc + 1) * 128], [], [b_wo[dc]], b_wo[dc])
        for g in range(4):
            c.dma("sp", xatt[:, g * 4:(g + 1) * 4, :], o_in[:, g * 4:(g + 1) * 4, t0:t0 + NT], [k.b_scr], [b_xatt], b_xatt)
        for g in range(2):
            c.dma("sp", xmem[:, g * 4:(g + 1) * 4, :], hm_in[:, g * 4:(g + 1) * 4, t0:t0 + NT], [k.b_scr], [b_xmem], b_xmem)
        for dc in range(16):
            sl = dc % 2
            c.dma("pool", wa[sl], wav[:, :, dc * 128:(dc + 1) * 128], [], [b_wa[sl]], b_wa[sl])
            c.dma("pool", wm[sl], wmv[:, :, dc * 128:(dc + 1) * 128], [], [b_wm[sl]], b_wm[sl])
            c.dma("sp", sga[sl], k.sga_scr[dc, :, t0:t0 + NT], [k.b_scr], [b_sga[sl]], b_sga[sl])
            c.dma("sp", sgm[sl], k.sgm_scr[dc, :, t0:t0 + NT], [k.b_scr], [b_sgm[sl]], b_sgm[sl])
            pset = sl * 4
            for hf in range(2):
                for kc in range(16):
                    c.mm(ps[pset + hf], wa[sl][:, kc, :], xatt[:, kc, hf * 512:(hf + 1) * 512], kc == 0, kc == 15,
                         [b_wa[sl], b_xatt], [b_ps[pset + hf]], inc=(kc == 15))
            for hf in range(2):
                for kc in range(8):
                    c.mm(ps[pset + 2 + hf], wm[sl][:, kc, :], xmem[:, kc, hf * 512:(hf + 1) * 512], kc == 0, kc == 7,
                         [b_wm[sl], b_xmem], [b_ps[pset + 2 + hf]], inc=(kc == 7))
            for hf in range(2):
                hs = slice(hf * 512, (hf + 1) * 512)
                c.tt("dve", t1[sl][:, hs], ps[pset + hf], sga[sl][:, hs], ALU.mult, [b_ps[pset + hf], b_sga[sl]], [b_t1[sl]])
                c.tt("dve", t2[sl][:, hs], ps[pset + 2 + hf], sgm[sl][:, hs], ALU.mult, [b_ps[pset + 2 + hf], b_sgm[sl]], [b_t2[sl]])
            c.tt("pool", mrg[:, dc, :], t1[sl], t2[sl], ALU.add, [b_t1[sl], b_t2[sl]], [b_mrg[dc]])
        for dc in range(16):
            sl = dc % 2
            if dc >= 2:
                c.dma("pool", wo[sl], wov[:, :, dc * 128:(dc + 1) * 128], [], [b_wo[sl]], b_wo[sl])
            c.dma("sp", hres[sl], k.hbuf[dc * 128:(dc + 1) * 128, t0:t0 + NT], [k.b_h], [b_hres[sl]], b_hres[sl])
            pset = sl * 2
            for hf in range(2):
                for kc in range(16):
                    c.mm(ps[pset + hf], wo[sl][:, kc, :], mrg[:, kc, hf * 512:(hf + 1) * 512], kc == 0, kc == 15,
                         [b_wo[sl], b_mrg[kc]], [b_ps[pset + hf]], inc=(kc == 15))
            for hf in range(2):
                hs = slice(hf * 512, (hf + 1) * 512)
                c.tt("dve", hres[sl][:, hs], ps[pset + hf], hres[sl][:, hs], ALU.add, [b_ps[pset + hf], b_hres[sl]], [b_hres[sl]])
            c.dma("sp", k.hbuf[dc * 128:(dc + 1) * 128, t0:t0 + NT], hres[sl], [b_hres[sl]], [k.b_h], b_hst[sl])
        c.barrier()
    c.pop_scope()
    ar.reset(m0)


def full_plan(n_layers=NL):
    plan = []
    for l in range(n_layers):
        plan.append(("ffn", l, 1, "x" if l == 0 else "h", "h"))
        plan += [("m1", l), ("m2", l), ("m3", l), ("m4", l)]
        plan.append(("ffn", l, 2, "h", "o" if l == n_layers - 1 else "h"))
    return plan


_WNAMES = ["ffn1_w_gu", "ffn1_w_down", "w_in", "w_att_out", "w_mem_out", "w_out", "ffn2_w_gu", "ffn2_w_down"]
_NC_CACHE = {}


def make_maps(inp, batch_ids):
    cst, tb, rb31 = host_consts(np.asarray(inp["rel_bias"], np.float32))
    cols = host_cols(inp)
    shared = {"cols": cols, "cst": cst, "tbraw": tb, "rb31": rb31}
    for n in _WNAMES:
        a = np.asarray(inp[n], np.float32)
        shared[n] = np.ascontiguousarray(a.reshape(NL, -1, a.shape[-1]))
    maps = []
    for b in batch_ids:
        m = dict(shared)
        m["xT"] = np.ascontiguousarray(np.asarray(inp["x"][b], np.float32).T)
        maps.append(m)
    return maps


def kernel(**inputs):
    if "nc" not in _NC_CACHE:
        _NC_CACHE["nc"] = build(full_plan(NL))
    nc = _NC_CACHE["nc"]
    B = inputs["x"].shape[0]
    maps = make_maps(inputs, list(range(B)))
    res = run_bass_kernel_spmd(nc, maps, core_ids=list(range(B)))
    out = np.stack([np.asarray(r["outT"]).T for r in res.results], axis=0)
    return np.ascontiguousarray(out.astype(np.float32))
```
